# Optimizing a Trainium2 kernel written in Bass

```python
import math
import jax, jax.numpy as jnp
from jax import lax
import numpy as np

D_MODEL = 1024
BATCH = 16
SEQ = 2048
DEPTH = 2

N_GROUPS = 4
GROUP_WIDTH = D_MODEL // N_GROUPS
MIX_WIDTH = N_GROUPS * GROUP_WIDTH
HEAD_DIM = 64
GROUP_HEADS = GROUP_WIDTH // HEAD_DIM
N_IN_SLICES = 10
ROPE_THETA = 10000.0
MOBA_BLOCK = 256
MOBA_TOPK = 3
MOBA_QCHUNK = 16
DILATED_CONFIGS = ((128, 1), (512, 4), (2048, 16))
GMLP_CHUNK = 128
GMLP_GROUPS = 4
CONV_WIDTH = 31
D_FF_DENSE = 2816
N_EXPERTS = 8
TOP_K = 2
D_FF_EXPERT = 3584
MOE_ROW_BLOCK = 256
N_DENSE_LAYERS = (DEPTH + 1) // 2
N_MOE_LAYERS = DEPTH // 2
NORM_EPS = 1e-6
NEG_INF = -1e30
ATTN_SCALE = HEAD_DIM ** -0.5

kernel_name = 'hybrid_moba_dilated_gmlp_conformer_moe_block'


def rmsnorm(x, g):
    xf = x.astype(jnp.float32)
    y = xf * lax.rsqrt(jnp.mean(xf * xf, axis=-1, keepdims=True) + NORM_EPS)
    return (y * g.astype(jnp.float32)).astype(x.dtype)


def layernorm(x):
    xf = x.astype(jnp.float32)
    mu = jnp.mean(xf, axis=-1, keepdims=True)
    var = jnp.mean(jnp.square(xf - mu), axis=-1, keepdims=True)
    return ((xf - mu) * lax.rsqrt(var + NORM_EPS)).astype(x.dtype)


def rope(x, positions):
    half = HEAD_DIM // 2
    inv_freq = jnp.power(ROPE_THETA, -jnp.arange(half, dtype=jnp.float32) / half)
    ang = positions.astype(jnp.float32)[..., None] * inv_freq
    cos = jnp.cos(ang)[:, :, None, :]
    sin = jnp.sin(ang)[:, :, None, :]
    x1 = x[..., :half].astype(jnp.float32)
    x2 = x[..., half:].astype(jnp.float32)
    return jnp.concatenate([x1 * cos - x2 * sin, x2 * cos + x1 * sin], axis=-1).astype(x.dtype)


def swiglu(x, w_gate, w_up, w_down):
    return (jax.nn.silu(x @ w_gate) * (x @ w_up)) @ w_down


def moba_attention(q, k, v):
    B, S, H, D = q.shape
    s_pad = -(-S // MOBA_BLOCK) * MOBA_BLOCK
    n_blk = s_pad // MOBA_BLOCK
    pad = ((0, 0), (0, s_pad - S), (0, 0), (0, 0))
    q, k, v = (jnp.pad(t, pad).transpose(0, 2, 1, 3) for t in (q, k, v))
    kb = k.reshape(B, H, n_blk, MOBA_BLOCK, D)
    vb = v.reshape(B, H, n_blk, MOBA_BLOCK, D)
    k_mean = jnp.mean(kb.astype(jnp.float32), axis=3)
    gate = jnp.einsum('bhsd,bhnd->bhsn', q.astype(jnp.float32), k_mean)
    q_blk = jnp.arange(s_pad) // MOBA_BLOCK
    past = jnp.arange(n_blk)[None, :] < q_blk[:, None]
    gate = jnp.where(past, gate, NEG_INF)
    n_sel = min(MOBA_TOPK, n_blk)
    _, sel = lax.top_k(gate, n_sel)
    sel_ok = sel < q_blk[:, None]
    n_chunks = s_pad // MOBA_QCHUNK

    def to_chunks(t):
        return jnp.moveaxis(t.reshape(B, H, n_chunks, MOBA_QCHUNK, *t.shape[3:]), 2, 0)

    b_idx = jnp.arange(B)[:, None, None, None]
    h_idx = jnp.arange(H)[None, :, None, None]
    key_off = jnp.arange(MOBA_BLOCK)
    q_off = jnp.arange(MOBA_QCHUNK)

    def chunk(args):
        qc, selc, okc, start = args
        blk = start // MOBA_BLOCK
        k_own = lax.dynamic_index_in_dim(kb, blk, axis=2, keepdims=False)
        v_own = lax.dynamic_index_in_dim(vb, blk, axis=2, keepdims=False)
        causal = (blk * MOBA_BLOCK + key_off)[None, :] <= (start + q_off)[:, None]
        s_own = jnp.einsum('bhqd,bhkd->bhqk', qc, k_own).astype(jnp.float32) * ATTN_SCALE
        s_own = jnp.where(causal, s_own, NEG_INF)
        k_g = kb[b_idx, h_idx, selc]
        v_g = vb[b_idx, h_idx, selc]
        s_sel = jnp.einsum('bhqd,bhqnkd->bhqnk', qc, k_g).astype(jnp.float32) * ATTN_SCALE
        s_sel = jnp.where(okc[..., None], s_sel, NEG_INF).reshape(B, H, MOBA_QCHUNK, n_sel * MOBA_BLOCK)
        p = jax.nn.softmax(jnp.concatenate([s_own, s_sel], axis=-1), axis=-1)
        p_own = p[..., :MOBA_BLOCK]
        p_sel = p[..., MOBA_BLOCK:].reshape(B, H, MOBA_QCHUNK, n_sel, MOBA_BLOCK)
        return (jnp.einsum('bhqk,bhkd->bhqd', p_own, v_own)
                + jnp.einsum('bhqnk,bhqnkd->bhqd', p_sel, v_g))

    starts = jnp.arange(n_chunks, dtype=jnp.int32) * MOBA_QCHUNK
    out = lax.map(chunk, (to_chunks(q), to_chunks(sel), to_chunks(sel_ok), starts))
    out = jnp.moveaxis(out, 0, 2).reshape(B, H, s_pad, D)[:, :, :S]
    return out.transpose(0, 2, 1, 3).astype(q.dtype)


def strided_window_attention(q, k, v, span, dil):
    B, S, H, D = q.shape
    L = S // dil
    blk = span
    l_pad = -(-L // blk) * blk
    nb = l_pad // blk

    def sub(t):
        t = t.reshape(B, L, dil, H, D).transpose(0, 2, 3, 1, 4)
        t = jnp.pad(t, ((0, 0), (0, 0), (0, 0), (0, l_pad - L), (0, 0)))
        return t.reshape(B, dil, H, nb, blk, D)

    def band_keys(t):
        prev = jnp.pad(t, ((0, 0), (0, 0), (0, 0), (1, 0), (0, 0), (0, 0)))[:, :, :, :-1]
        return jnp.concatenate([prev, t], axis=4)

    qs = sub(q)
    k_span = band_keys(sub(k))
    v_span = band_keys(sub(v))
    qi = jnp.arange(blk)[:, None]
    kj = jnp.arange(2 * blk)[None, :]
    rel = blk + qi - kj
    band = (rel >= 0) & (rel <= span)
    not_first = jnp.arange(nb)[:, None, None] > 0
    mask = band[None] & (not_first | (kj >= blk)[None])
    s = jnp.einsum('bghnqd,bghnkd->bghnqk', qs, k_span).astype(jnp.float32) * ATTN_SCALE
    s = jnp.where(mask, s, NEG_INF)
    m = jnp.max(s, axis=-1, keepdims=True)
    p = jnp.exp(s - m)
    den = jnp.sum(p, axis=-1, keepdims=True)
    o = jnp.einsum('bghnqk,bghnkd->bghnqd', p, v_span) / den
    lse = (m + jnp.log(den))[..., 0]
    o = o.reshape(B, dil, H, l_pad, D)[:, :, :, :L].transpose(0, 3, 1, 2, 4).reshape(B, S, H, D)
    lse = lse.reshape(B, dil, H, l_pad)[..., :L].transpose(0, 3, 1, 2).reshape(B, S, H)
    return o, lse


def dilated_attention(q, k, v):
    outs, lses = zip(*[strided_window_attention(q, k, v, w // d, d) for (w, d) in DILATED_CONFIGS])
    o = jnp.stack(outs)
    wts = jax.nn.softmax(jnp.stack(lses), axis=0)
    return jnp.sum(wts[..., None] * o, axis=0).astype(q.dtype)


def spatial_gating(u, v, ws, b):
    B, S, C = v.shape
    n_chunk = S // GMLP_CHUNK
    vg = layernorm(v).reshape(B, n_chunk, GMLP_CHUNK, GMLP_GROUPS, C // GMLP_GROUPS)
    causal = jnp.tril(jnp.ones((GMLP_CHUNK, GMLP_CHUNK), dtype=bool))
    w = jnp.where(causal, ws, 0)
    z = jnp.einsum('gts,bnsgc->bntgc', w, vg) + b.T[:, :, None]
    return u * z.reshape(B, S, C)


def conv_module(a, g, conv_w, conv_b, ln_g, ln_b):
    x = a * jax.nn.sigmoid(g)
    C = x.shape[-1]
    y = lax.conv_general_dilated(x, conv_w[:, None, :], window_strides=(1,),
                                 padding=((CONV_WIDTH - 1, 0),),
                                 dimension_numbers=('NWC', 'WIO', 'NWC'),
                                 feature_group_count=C) + conv_b
    y = layernorm(y) * ln_g + ln_b
    return jax.nn.silu(y)


def hybrid_mixer(h, positions, w_in, w_out, group_g, gmlp_ws, gmlp_b,
                 conv_w, conv_b, conv_ln_g, conv_ln_b):
    B, S, _ = h.shape
    qa, ka, va, qb, kb, vb, u, vg, ga, gg = jnp.split(h @ w_in, N_IN_SLICES, axis=-1)

    def heads(t):
        return t.reshape(B, S, GROUP_HEADS, HEAD_DIM)

    o_a = moba_attention(rope(heads(qa), positions), rope(heads(ka), positions), heads(va))
    o_b = dilated_attention(rope(heads(qb), positions), rope(heads(kb), positions), heads(vb))
    o_c = spatial_gating(jax.nn.gelu(u), jax.nn.gelu(vg), gmlp_ws, gmlp_b)
    o_d = conv_module(ga, gg, conv_w, conv_b, conv_ln_g, conv_ln_b)
    mix = jnp.concatenate([o_a.reshape(B, S, GROUP_WIDTH), o_b.reshape(B, S, GROUP_WIDTH), o_c, o_d], axis=-1)
    mix = rmsnorm(mix.reshape(B, S, N_GROUPS, GROUP_WIDTH), group_g).reshape(B, S, MIX_WIDTH)
    return mix @ w_out


def moe_swiglu(h, router_w, router_b, w_gate, w_up, w_down):
    B, S, D = h.shape
    n_tok = B * S
    n_assign = n_tok * TOP_K
    xt = h.reshape(n_tok, D)
    logits = (xt @ router_w).astype(jnp.float32) + router_b.astype(jnp.float32)
    top_logit, top_idx = lax.top_k(logits, TOP_K)
    gates = jax.nn.softmax(top_logit, axis=-1).reshape(n_assign)
    expert = top_idx.reshape(n_assign)
    order = jnp.argsort(expert)
    e_sorted = expert[order]
    tok_sorted = order // TOP_K
    g_sorted = gates[order]
    counts = jnp.bincount(expert, length=N_EXPERTS)
    starts = jnp.cumsum(counts) - counts
    padded = (counts + MOE_ROW_BLOCK - 1) // MOE_ROW_BLOCK * MOE_ROW_BLOCK
    pad_end = jnp.cumsum(padded)
    pad_start = pad_end - padded
    dest = pad_start[e_sorted] + jnp.arange(n_assign) - starts[e_sorted]
    n_blocks = -(-n_assign // MOE_ROW_BLOCK) + N_EXPERTS
    rows = jnp.zeros((n_blocks * MOE_ROW_BLOCK, D), h.dtype).at[dest].set(xt[tok_sorted])
    block_expert = jnp.clip(jnp.searchsorted(pad_end, jnp.arange(n_blocks) * MOE_ROW_BLOCK, side='right'),
                            0, N_EXPERTS - 1)

    def run_block(args):
        xb, e = args
        return swiglu(xb, w_gate[e], w_up[e], w_down[e])

    out = lax.map(run_block, (rows.reshape(n_blocks, MOE_ROW_BLOCK, D), block_expert)).reshape(-1, D)
    y = jax.ops.segment_sum(out[dest] * g_sorted[:, None], tok_sorted, num_segments=n_tok)
    return y.reshape(B, S, D).astype(h.dtype)


def setup_inputs(seed: int = 0) -> dict:
    key = jax.random.key(seed)
    ks = jax.random.split(key, 26)

    def nrm(k, shape, scale):
        return jax.random.normal(k, shape, jnp.float32) * scale

    def gain(k, shape):
        return 1.0 + 0.05 * jax.random.normal(k, shape, jnp.float32)

    positions = (jax.random.randint(ks[2], (BATCH, 1), 0, 4096, dtype=jnp.int32)
                 + jnp.arange(SEQ, dtype=jnp.int32)[None, :])
    return {
        'x': nrm(ks[0], (BATCH, SEQ, D_MODEL), 1.0),
        'c': nrm(ks[1], (BATCH, D_MODEL), 1.0),
        'positions': positions,
        'ada_w': nrm(ks[3], (DEPTH, D_MODEL, 6 * D_MODEL), 0.5 * D_MODEL ** -0.5),
        'ada_b': nrm(ks[4], (DEPTH, 6 * D_MODEL), 0.02),
        'mix_pre_g': gain(ks[5], (DEPTH, D_MODEL)),
        'mix_post_g': gain(ks[6], (DEPTH, D_MODEL)),
        'ffn_pre_g': gain(ks[7], (DEPTH, D_MODEL)),
        'ffn_post_g': gain(ks[8], (DEPTH, D_MODEL)),
        'w_in': nrm(ks[9], (DEPTH, D_MODEL, N_IN_SLICES * GROUP_WIDTH), D_MODEL ** -0.5),
        'w_out': nrm(ks[10], (DEPTH, MIX_WIDTH, D_MODEL), MIX_WIDTH ** -0.5),
        'group_out_g': gain(ks[11], (DEPTH, N_GROUPS, GROUP_WIDTH)),
        'gmlp_ws': nrm(ks[12], (DEPTH, GMLP_GROUPS, GMLP_CHUNK, GMLP_CHUNK), GMLP_CHUNK ** -0.5),
        'gmlp_b': gain(ks[13], (DEPTH, GMLP_GROUPS, GMLP_CHUNK)),
        'conv_w': nrm(ks[14], (DEPTH, CONV_WIDTH, GROUP_WIDTH), CONV_WIDTH ** -0.5),
        'conv_b': nrm(ks[15], (DEPTH, GROUP_WIDTH), 0.02),
        'conv_ln_g': gain(ks[16], (DEPTH, GROUP_WIDTH)),
        'conv_ln_b': nrm(ks[17], (DEPTH, GROUP_WIDTH), 0.02),
        'ffn_w_gate': nrm(ks[18], (N_DENSE_LAYERS, D_MODEL, D_FF_DENSE), D_MODEL ** -0.5),
        'ffn_w_up': nrm(ks[19], (N_DENSE_LAYERS, D_MODEL, D_FF_DENSE), D_MODEL ** -0.5),
        'ffn_w_down': nrm(ks[20], (N_DENSE_LAYERS, D_FF_DENSE, D_MODEL), D_FF_DENSE ** -0.5),
        'router_w': nrm(ks[21], (N_MOE_LAYERS, D_MODEL, N_EXPERTS), D_MODEL ** -0.5),
        'router_b': nrm(ks[22], (N_MOE_LAYERS, N_EXPERTS), 0.01),
        'moe_w_gate': nrm(ks[23], (N_MOE_LAYERS, N_EXPERTS, D_MODEL, D_FF_EXPERT), D_MODEL ** -0.5),
        'moe_w_up': nrm(ks[24], (N_MOE_LAYERS, N_EXPERTS, D_MODEL, D_FF_EXPERT), D_MODEL ** -0.5),
        'moe_w_down': nrm(ks[25], (N_MOE_LAYERS, N_EXPERTS, D_FF_EXPERT, D_MODEL), D_FF_EXPERT ** -0.5),
    }


def reference(x, c, positions, ada_w, ada_b, mix_pre_g, mix_post_g, ffn_pre_g, ffn_post_g,
              w_in, w_out, group_out_g, gmlp_ws, gmlp_b, conv_w, conv_b, conv_ln_g, conv_ln_b,
              ffn_w_gate, ffn_w_up, ffn_w_down, router_w, router_b, moe_w_gate, moe_w_up, moe_w_down):
    c_act = jax.nn.silu(c)
    for layer in range(DEPTH):
        mod = c_act @ ada_w[layer] + ada_b[layer]
        sh_m, sc_m, gt_m, sh_f, sc_f, gt_f = (t[:, None, :] for t in jnp.split(mod, 6, axis=-1))
        h = rmsnorm(x, mix_pre_g[layer]) * (1 + sc_m) + sh_m
        y = hybrid_mixer(h, positions, w_in[layer], w_out[layer], group_out_g[layer],
                         gmlp_ws[layer], gmlp_b[layer], conv_w[layer], conv_b[layer],
                         conv_ln_g[layer], conv_ln_b[layer])
        x = x + gt_m * rmsnorm(y, mix_post_g[layer])
        h = rmsnorm(x, ffn_pre_g[layer]) * (1 + sc_f) + sh_f
        i = layer // 2
        if layer % 2 == 0:
            y = swiglu(h, ffn_w_gate[i], ffn_w_up[i], ffn_w_down[i])
        else:
            y = moe_swiglu(h, router_w[i], router_b[i], moe_w_gate[i], moe_w_up[i], moe_w_down[i])
        x = x + gt_f * rmsnorm(y, ffn_post_g[layer])
    return x
```

```python
from contextlib import ExitStack
import math
import numpy as np
import concourse.bass as bass
import concourse.mybir as mybir
from concourse.bass_utils import run_bass_kernel_spmd

F32 = mybir.dt.float32
BF16 = mybir.dt.bfloat16
I32 = mybir.dt.int32
AF = mybir.ActivationFunctionType
ALU = mybir.AluOpType
AX = mybir.AxisListType

S = 2048
D = 1024
NCORES = 8
NSEQ = 2
DFF = 2816
DFE = 3584
NE = 8
EPS = 1e-6
BIG = 30000.0
TWO_PI = 2.0 * math.pi

ENGS = ('pe', 'act', 'dve', 'pool', 'sp')
N_DMA_SEMS = 40
N_SP_SEMS = 8


class Buf:
    def __init__(self, name, t=None):
        self.name = name
        self.t = t
        self.last_w = None
        self.readers = {}


class _Op:
    __slots__ = ('eng', 'fn', 'deps', 'idx', 'needed', 'dma', 'sem', 'val', 'cnt')

    def __init__(self, eng, fn, dma):
        self.eng = eng
        self.fn = fn
        self.deps = []
        self.needed = False
        self.dma = dma
        self.sem = None
        self.val = 0
        self.cnt = 0


class Prog:
    def __init__(self, nc):
        self.nc = nc
        self.stack = ExitStack()
        self.ops = {e: [] for e in ENGS}
        self.n_dma = 0
        self.n_dma_sp = 0
        self.dma_last = [None] * N_DMA_SEMS
        self.dma_uses = [0] * N_DMA_SEMS
        self.pending = {e: [] for e in ENGS}

    def sb(self, name, shape, dt):
        t = self.stack.enter_context(self.nc.sbuf_tensor(name, list(shape), dt))
        return Buf(name, t)

    def ps(self, name, shape, dt):
        t = self.stack.enter_context(self.nc.psum_tensor(name, list(shape), dt))
        return Buf(name, t)

    def _add(self, eng, fn, reads, writes, dma=False):
        op = _Op(eng, fn, dma)
        op.idx = len(self.ops[eng])
        deps = list(self.pending[eng])
        self.pending[eng] = []
        for b in reads:
            if b.last_w is not None:
                deps.append(b.last_w)
        for b in writes:
            if b.last_w is not None:
                deps.append(b.last_w)
            deps.extend(b.readers.values())
        for b in reads:
            b.readers[('dma', id(op)) if dma else eng] = op
        for b in writes:
            b.last_w = op
            b.readers = {}
        seen = set()
        for d in deps:
            if d is op or id(d) in seen:
                continue
            seen.add(id(d))
            if (not d.dma) and (not dma) and d.eng == eng == 'pe':
                continue
            op.deps.append(d)
            d.needed = True
        self.ops[eng].append(op)
        return op

    def op(self, eng, fn, reads=(), writes=()):
        return self._add(eng, fn, reads, writes)

    def _dma(self, eng, fn, reads, writes):
        op = self._add(eng, fn, reads, writes, dma=True)
        if eng == 'sp':
            k = self.n_dma_sp % N_SP_SEMS
            self.n_dma_sp += 1
        else:
            k = N_SP_SEMS + self.n_dma % (N_DMA_SEMS - N_SP_SEMS)
            self.n_dma += 1
        prev = self.dma_last[k]
        if prev is not None:
            op.deps.append(prev)
        self.dma_uses[k] += 1
        op.sem = k
        op.val = 16 * self.dma_uses[k]
        self.dma_last[k] = op
        return op

    def dma(self, out, in_, reads=(), writes=()):
        return self._dma('sp', lambda e: e.dma_start(out=out, in_=in_), reads, writes)

    def dma_cast(self, out, in_, reads=(), writes=()):
        return self._dma('pool', lambda e: e.dma_start(out=out, in_=in_), reads, writes)

    def barrier(self):
        lasts = [o for o in self.dma_last if o is not None]
        for e in ENGS:
            for o in reversed(self.ops[e]):
                if not o.dma:
                    o.needed = True
                    lasts.append(o)
                    break
        for e in ENGS:
            self.pending[e] = list(lasts)

    def finish(self):
        nc = self.nc
        self.barrier()
        fin = _Op('sp', None, False)
        fin.deps = list(self.pending['sp'])
        self.ops['sp'].append(fin)
        for e in ENGS:
            c = 0
            for o in self.ops[e]:
                if (not o.dma) and o.needed:
                    c += 1
                    o.cnt = c
        st = self.stack
        esem = {e: st.enter_context(nc.semaphore("s_" + e)) for e in ENGS}
        dsem = [st.enter_context(nc.semaphore("d%d" % k)) for k in range(N_DMA_SEMS)]
        block = st.enter_context(nc.Block())
        ops = self.ops

        def run(ename, eng):
            waited = {}
            for o in ops[ename]:
                need = {}
                for d in o.deps:
                    if d.dma:
                        key = ('d', d.sem)
                        v = d.val
                    else:
                        key = ('e', d.eng)
                        v = d.cnt
                    if v > need.get(key, 0):
                        need[key] = v
                for key, v in need.items():
                    if waited.get(key, 0) >= v:
                        continue
                    waited[key] = v
                    eng.wait_ge(dsem[key[1]] if key[0] == 'd' else esem[key[1]], v)
                if o.fn is None:
                    continue
                ins = o.fn(eng)
                if o.dma:
                    ins.then_inc(dsem[o.sem], 16)
                elif o.needed:
                    ins.then_inc(esem[ename], 1)

        @block.tensor
        def _(eng):
            run('pe', eng)

        @block.scalar
        def _(eng):
            run('act', eng)

        @block.vector
        def _(eng):
            run('dve', eng)

        @block.gpsimd
        def _(eng):
            run('pool', eng)

        @block.sync
        def _(eng):
            run('sp', eng)

        st.close()


_DTSIZE = {F32: 4, BF16: 2, I32: 4}


class Arena:
    def __init__(self, P, name, nwords):
        self.t = P.sb(name, [128, nwords], F32).t
        self.cap = nwords
        self.off = 0

    def reset(self):
        self.off = 0

    def carve(self, name, shape, dt, parts=128):
        n = 1
        for s_ in shape:
            n *= s_
        words = (n * _DTSIZE[dt] + 3) // 4
        assert self.off + words <= self.cap, (name, self.off, words, self.cap)
        ap = self.t[0:parts, self.off:self.off + words]
        if dt != F32:
            ap = ap.bitcast(dt)
        if len(shape) == 2:
            ap = ap.rearrange("p (a b) -> p a b", a=shape[0])
        elif len(shape) == 3:
            ap = ap.rearrange("p (a b c) -> p a b c", a=shape[0], b=shape[1])
        self.off += words
        return Buf(name, ap)


def MM(P, ob, o, a, b, rd, st=True, sp=True):
    P.op('pe', lambda e: e.matmul(o, a, b, start=st, stop=sp), rd, [ob])


def TR(P, ob, o, a, ident, rd):
    P.op('pe', lambda e: e.transpose(o, a, ident), rd, [ob])


def ACT(P, ob, o, i, f, rd, bias=0.0, scale=1.0, acc=None, wr=None):
    if acc is None:
        P.op('act', lambda e: e.activation(o, i, f, bias=bias, scale=scale), rd, wr or [ob])
    else:
        P.op('act', lambda e: e.activation(o, i, f, bias=bias, scale=scale, accum_out=acc), rd, wr or [ob])


def TT(P, eng, ob, o, a, b, op, rd):
    P.op(eng, lambda e: e.tensor_tensor(o, a, b, op), rd, [ob])


def TS(P, eng, ob, o, a, s1, s2, op0, op1, rd):
    if op1 is None:
        P.op(eng, lambda e: e.tensor_scalar(o, a, s1, None, op0), rd, [ob])
    else:
        P.op(eng, lambda e: e.tensor_scalar(o, a, s1, s2, op0, op1), rd, [ob])


def STT(P, eng, ob, o, a, s, b, op0, op1, rd):
    P.op(eng, lambda e: e.scalar_tensor_tensor(o, a, s, b, op0, op1), rd, [ob])


def CP(P, eng, ob, o, i, rd):
    P.op(eng, lambda e: e.tensor_copy(o, i), rd, [ob])


def _cmap():
    m = {}
    off = 0
    for name, n in (('invf', 1), ('sgn', 1), ('npsgn', 1), ('negpi', 1), ('gains', 64), ('ggain', 16),
                    ('gb', 8), ('convw', 124), ('convv', 12), ('ada_b', 96), ('rw', 64),
                    ('pastneg', 256), ('past', 256), ('own', 256), ('cT', 8 * NSEQ), ('ones', 64)):
        m[name] = (off, n)
        off += n
    return m, off


CM, NCF = _cmap()


class K:
    def __init__(self, phases, dbg=False):
        self.phases = phases
        nc = bass.Bass("TRN2", target_bir_lowering=False)
        self.nc = nc
        P = Prog(nc)
        self.P = P

        def din(name, shape, dt=F32):
            return nc.dram_tensor(name, list(shape), dt, kind="ExternalInput").ap()

        self.xin = din("xT", [NSEQ, D, S])
        self.xout = nc.dram_tensor("outT", [NSEQ, D, S], F32, kind="ExternalOutput").ap()
        self.posrep = din("posrep", [NSEQ, 128, S], I32)
        self.cf_d = din("cf", [128, NCF])
        self.mB_d = din("mB", [128, 9 * 512])
        self.mA_d = din("mA", [128, 4 * 512])
        self.tri_d = din("tri", [128, 128])
        self.indc_d = din("indc", [8, 1024])
        self.rb_d = din("rbrep", [128, 8])
        self.ada_w = din("ada_w", [2, D, 6 * D])
        self.w_in = din("w_in", [2, D, 3584])
        self.w_out = din("w_out", [2, D, D])
        self.wsT = din("wsT", [2, 4, 128, 128])
        self.ffn_wg = din("ffn_wg", [D, DFF])
        self.ffn_wu = din("ffn_wu", [D, DFF])
        self.ffn_wd = din("ffn_wd", [DFF, D])
        self.moe_wg = din("moe_wg", [NE, D, DFE])
        self.moe_wu = din("moe_wu", [NE, D, DFE])
        self.moe_wd = din("moe_wd", [NE, DFE, D])
        self.dbg = {}
        self.dbg_on = dbg

        self.xT = P.sb("xTs", [128, 8, S], F32)
        self.xTt = [Buf("xTt%d" % i, self.xT.t) for i in range(4)]
        self.cf = P.sb("cfs", [128, NCF], F32)
        self.ident = P.sb("ident", [128, 128], BF16)
        self.identf = P.sb("identf", [128, 128], F32)
        self.ones = P.sb("ones", [128, 128], BF16)
        self.mB = P.sb("mBs", [128, 9, 512], BF16)
        self.mA = P.sb("mAs", [128, 4, 512], BF16)
        self.tri = P.sb("tris", [128, 128], BF16)
        self.indc = P.sb("indcs", [8, 1024], BF16)
        self.rb = P.sb("rbs", [128, 8], F32)
        self.cact = P.sb("cact", [128, 8, NSEQ], BF16)
        self.modv = P.sb("modv", [128, 2, 48, NSEQ], F32)
        self.drv = P.sb("drv", [128, 2, NSEQ, 4, 8], F32)
        self.rwb = P.sb("rwb", [128, 8, 8], BF16)
        self.PB = [P.ps("pb%d" % i, [128, 512], F32) for i in range(8)]
        self.ar = Arena(P, "arena", 30000)
        self.rot = {}

    def c(self, name, a=0, n=None):
        off, nn = CM[name]
        if n is None:
            n = nn - a
        return self.cf.t[:, off + a:off + a + n]

    def nxt(self, key, n):
        v = self.rot.get(key, 0)
        self.rot[key] = (v + 1) % n
        return v

    def tap(self, name, buf, ap, shape, dt=F32):
        if not self.dbg_on or name in self.dbg:
            return
        d = self.nc.dram_tensor("dbg_" + name, list(shape), dt, kind="ExternalOutput").ap()
        self.dbg[name] = d
        self.P.dma(d, ap, reads=[buf])

    def setup(self):
        P = self.P
        P.dma(self.cf.t[:], self.cf_d, writes=[self.cf])
        P.dma(self.rb.t[:], self.rb_d, writes=[self.rb])
        P.dma_cast(self.mB.t[:], self.mB_d.rearrange("p (a b) -> p a b", a=9), writes=[self.mB])
        P.dma_cast(self.mA.t[:], self.mA_d.rearrange("p (a b) -> p a b", a=4), writes=[self.mA])
        P.dma_cast(self.tri.t[:], self.tri_d, writes=[self.tri])
        P.dma_cast(self.indc.t[:], self.indc_d, writes=[self.indc])
        for idb, val in ((self.ident, 1.0), (self.identf, 1.0)):
            P.op('pool', lambda e, t=idb.t: e.memset(t[:], 1.0), (), [idb])
            P.op('pool', lambda e, t=idb.t: e.affine_select(t[:], t[:], [[-1, 128]], ALU.is_equal, 0.0, base=0,
                                                            channel_multiplier=1), [idb], [idb])
        P.op('pool', lambda e: e.memset(self.ones.t[:], 1.0), (), [self.ones])
        CP(P, 'dve', self.rwb, self.rwb.t[:], self.c('rw').rearrange("p (a b) -> p a b", a=8), [self.cf])
        ACT(P, self.cact, self.cact.t[:], self.c('cT').rearrange("p (a b) -> p a b", a=8), AF.Silu, [self.cf])
        ar = self.ar
        ar.reset()
        wb = [ar.carve("wada%d" % i, [8, 512], BF16) for i in range(2)]
        self.mod_layer(0, wb, 512)
        self.mod_deferred = (('ffn', 0) in self.phases) and (('mix', 1) in self.phases or ('ffn', 1) in self.phases) \
            and self.phases.index(('ffn', 0)) < min(self.phases.index(p) for p in self.phases if p[1] == 1)
        if not self.mod_deferred:
            self.mod_layer(1, wb, 512)
        P.barrier()

    def mod_layer(self, l, wb, W):
        for _ in self.mod_layer_gen(l, wb, W):
            pass

    def mod_layer_gen(self, l, wb, W):
        P = self.P
        pm = self.PB[0]
        nsub = W // 128
        nblk = 6 * D // W

        def load(blk):
            w = wb[blk % 2]
            P.dma_cast(w.t[:, :, 0:W], self.ada_w[l, :, blk * W:(blk + 1) * W].rearrange("(kc p) n -> p kc n", p=128),
                       writes=[w])

        load(0)
        for blk in range(nblk):
            if blk + 1 < nblk:
                load(blk + 1)
            yield
            w = wb[blk % 2]
            for oc in range(nsub):
                j = blk * nsub + oc
                o = pm.t[:, oc * NSEQ:(oc + 1) * NSEQ]
                for kc in range(8):
                    MM(P, pm, o, w.t[:, kc, oc * 128:(oc + 1) * 128], self.cact.t[:, kc, :], [w, self.cact],
                       kc == 0, kc == 7)
                ACT(P, self.modv, self.modv.t[:, l, j, :], o, AF.Identity, [pm, self.cf],
                    bias=self.c('ada_b', l * 48 + j, 1))
        for b in range(NSEQ):
            g = lambda w_: self.c('gains', (l * 4 + w_) * 8, 8)
            d = self.drv
            STT(P, 'dve', d, d.t[:, l, b, 0, :], self.modv.t[:, l, 8:16, b], 1.0, g(0), ALU.add, ALU.mult,
                [self.modv, self.cf])
            TT(P, 'dve', d, d.t[:, l, b, 1, :], self.modv.t[:, l, 16:24, b], g(1), ALU.mult, [self.modv, self.cf])
            STT(P, 'dve', d, d.t[:, l, b, 2, :], self.modv.t[:, l, 32:40, b], 1.0, g(2), ALU.add, ALU.mult,
                [self.modv, self.cf])
            TT(P, 'dve', d, d.t[:, l, b, 3, :], self.modv.t[:, l, 40:48, b], g(3), ALU.mult, [self.modv, self.cf])

    def rsq(self, buf, ap):
        P = self.P
        ACT(P, buf, ap, ap, AF.Sqrt, [buf])
        P.op('dve', lambda e: e.reciprocal(ap, ap), [buf], [buf])

    def rms_rstd(self, get_src, n, W, sqb, rstd):
        P = self.P
        pb = self.PB[0]
        for c in range(n):
            sb_, sap = get_src(c)
            q = sqb[c % 2]
            ACT(P, q, q.t[:, 0:W], sap, AF.Square, [sb_])
            MM(P, pb, pb.t[:, 0:W], self.ones.t[:], q.t[:, 0:W], [q, self.ones], c == 0, c == n - 1)
        TS(P, 'dve', rstd, rstd.t[:, 0:W], pb.t[:, 0:W], 1.0 / (128 * n), EPS, ALU.mult, ALU.add, [pb])
        self.rsq(rstd, rstd.t[:, 0:W])

    def prenorm(self, s, l, which, t0, hm, hoff, sqb, rstd, tmp):
        P = self.P
        xT = self.xTt[t0 // 512]
        self.rms_rstd(lambda c: (xT, xT.t[:, c, t0:t0 + 512]), 8, 512, sqb, rstd)
        Aidx = 0 if which == 0 else 2
        shbase = 0 if which == 0 else 24
        for c in range(8):
            tb = tmp[c % 2]
            STT(P, 'dve', tb, tb.t[:], xT.t[:, c, t0:t0 + 512], self.drv.t[:, l, s, Aidx, c:c + 1], rstd.t[:],
                ALU.mult, ALU.mult, [xT, self.drv, rstd])
            ACT(P, hm, hm.t[:, c, hoff:hoff + 512], tb.t[:], AF.Identity, [tb, self.modv],
                bias=self.modv.t[:, l, shbase + c, s:s + 1])

    def postnorm_residual(self, s, l, which, t0, get_y, W, sqb, rstd, tmp):
        P = self.P
        xT = self.xTt[t0 // 512]
        self.rms_rstd(get_y, 8, W, sqb, rstd)
        Gidx = 1 if which == 0 else 3
        for c in range(8):
            tb = tmp[c % 2]
            yb, yap = get_y(c)
            TT(P, 'dve', tb, tb.t[:, 0:W], yap, rstd.t[:, 0:W], ALU.mult, [yb, rstd])
            STT(P, 'dve', xT, xT.t[:, c, t0:t0 + W], tb.t[:, 0:W], self.drv.t[:, l, s, Gidx, c:c + 1],
                xT.t[:, c, t0:t0 + W], ALU.mult, ALU.add, [tb, self.drv, xT])

    def group_norm_tm(self, l, src, src_ap, chunk0, mixT, tokoff, tmps):
        P = self.P
        junk, ssq, on = tmps
        ACT(P, junk, junk.t[:, 256:512], src_ap, AF.Square, [src], acc=ssq.t[:, 0:1], wr=[junk, ssq])
        TS(P, 'dve', ssq, ssq.t[:, 1:2], ssq.t[:, 0:1], 1.0 / 256, EPS, ALU.mult, ALU.add, [ssq])
        self.rsq(ssq, ssq.t[:, 1:2])
        TS(P, 'dve', on, on.t[:, 0:256], src_ap, ssq.t[:, 1:2], None, ALU.mult, None, [src, ssq])
        pt = self.PB[7]
        ptv = pt.t[:].bitcast(BF16)
        for cc in range(2):
            TR(P, pt, ptv[:, cc * 128:(cc + 1) * 128], on.t[:, cc * 128:(cc + 1) * 128], self.ident.t[:],
               [on, self.ident])
            ACT(P, mixT, mixT.t[:, chunk0 + cc, tokoff:tokoff + 128], ptv[:, cc * 128:(cc + 1) * 128], AF.Identity,
                [pt, self.cf], scale=self.c('ggain', l * 8 + chunk0 + cc, 1))

    def mixer(self, s, l):
        P = self.P
        ar = self.ar
        ar.reset()
        cv = ar.carve
        hm = cv("hm", [8, 512], BF16)
        mixT = cv("mixT", [8, 512], BF16)
        wreg = cv("wreg", [8, 1024], BF16)
        wblk = [Buf("wblk0", wreg.t[:, :, 0:512]), Buf("wblk1", wreg.t[:, :, 512:1024])]
        qT = cv("qT", [4, 512], BF16)
        kT = cv("kT", [4, S], BF16)
        vaug = cv("vaug", [16, 8, 65], BF16)
        glu = cv("glu", [2, 542], F32)
        biasT = cv("biasT", [4, 512], BF16, parts=8)
        posi = cv("posi", [512], I32)
        cosT = cv("cosT", [512], F32)
        sinS = cv("sinS", [512], F32)
        sqb = [cv("sq%d" % i, [512], BF16) for i in range(2)]
        rstd = cv("rstd", [512], F32)
        tmp = [cv("tmp%d" % i, [512], F32) for i in range(4)]
        PT = [cv("PT%d" % i, [512], BF16) for i in range(5)]
        gl = cv("gl", [512], F32)
        vgn = cv("vgn", [256], BF16)
        wsTm = cv("wsTm", [4, 128], BF16)
        bb = cv("bb", [256], F32)
        stt = cv("stt", [8], F32)
        ssq = cv("ssq", [4], F32)
        on = cv("on", [256], BF16)
        ocb = cv("ocb", [256], F32)
        og = cv("og", [4, 256], F32)
        rec = cv("rec", [4], F32)
        kms = cv("kms", [2, 8], F32)
        kmb = cv("kmb", [2, 8], BF16)
        gm = cv("gm", [32], F32)
        top8 = cv("top8", [4, 8], F32)
        sel = cv("sel", [32], F32)
        bq = cv("bq", [32], BF16)
        acc = [cv("acc%d" % i, [512], F32) for i in range(2)]
        ybf = cv("ybf", [2, 512], BF16)
        PB = self.PB
        cf = self.cf
        gnt = (gl, ssq, on)

        P.dma_cast(wsTm.t[:], self.wsT[l].rearrange("g s t -> s g t"), writes=[wsTm])
        for g in range(4):
            TT(P, 'dve', wsTm, wsTm.t[:, g, :], wsTm.t[:, g, :], self.tri.t[:], ALU.mult, [wsTm, self.tri])
            TS(P, 'dve', bb, bb.t[:, g * 64:(g + 1) * 64], self.c('ones'), self.c('gb', l * 4 + g, 1), None, ALU.mult,
               None, [cf])
        P.op('dve', lambda e: e.memset(vaug.t[:, :, :, 64:65], 1.0), (), [vaug])
        P.op('dve', lambda e: e.memset(glu.t[:, :, 0:30], 0.0), (), [glu])
        P.op('dve', lambda e: e.memset(kms.t[:], 0.0), (), [kms])

        wsrc = self.w_in[l]
        state = {'issued': 0}

        def issue_to(n):
            while state['issued'] < n:
                i = state['issued']
                b_ = i % 7
                w = wblk[i % 2]
                P.dma_cast(w.t[:], wsrc[:, b_ * 512:(b_ + 1) * 512].rearrange("(kc p) n -> p kc n", p=128), writes=[w])
                state['issued'] += 1

        for tt in range(4):
            t0 = tt * 512
            posf, angt = tmp[2], tmp[3]
            fi_, gg_ = tmp[0], tmp[1]
            posi2 = Buf("posi2", posi.t)
            P.dma(posi.t[:], self.posrep[s, :, t0:t0 + 512], writes=[posi])
            CP(P, 'dve', posf, posf.t[:], posi.t[:], [posi])
            TS(P, 'dve', posf, posf.t[:], posf.t[:], self.c('invf'), None, ALU.mult, None, [posf, cf])
            CP(P, 'dve', posi, posi.t[:], posf.t[:], [posf])
            CP(P, 'dve', fi_, fi_.t[:], posi.t[:], [posi])
            TT(P, 'dve', angt, angt.t[:], posf.t[:], fi_.t[:], ALU.subtract, [posf, fi_])
            TS(P, 'dve', gg_, gg_.t[:], angt.t[:], 0.5, None, ALU.is_gt, None, [angt])
            TT(P, 'dve', angt, angt.t[:], angt.t[:], gg_.t[:], ALU.subtract, [angt, gg_])
            TS(P, 'dve', gg_, gg_.t[:], angt.t[:], -0.5, None, ALU.is_lt, None, [angt])
            TT(P, 'dve', angt, angt.t[:], angt.t[:], gg_.t[:], ALU.add, [angt, gg_])
            ACT(P, sinS, sinS.t[:], angt.t[:], AF.Sin, [angt, cf], scale=self.c('sgn'))
            TS(P, 'dve', angt, angt.t[:], angt.t[:], 0.25, None, ALU.add, None, [angt])
            TS(P, 'dve', gg_, gg_.t[:], angt.t[:], 0.5, None, ALU.is_gt, None, [angt])
            TT(P, 'dve', angt, angt.t[:], angt.t[:], gg_.t[:], ALU.subtract, [angt, gg_])
            ACT(P, cosT, cosT.t[:], angt.t[:], AF.Sin, [angt], scale=6.28318)
            self.prenorm(s, l, 0, t0, hm, 0, sqb, rstd, tmp)
            if self.dbg_on and tt == 0:
                self.tap("cos", cosT, cosT.t[:], [128, 512])
                self.tap("sin", sinS, sinS.t[:], [128, 512])
            for b in range(7):
                bi = tt * 7 + b
                issue_to(min(bi + 2, (tt + 1) * 7))
                w = wblk[bi % 2]
                if b < 5:
                    for cc in range(2):
                        pa = PB[1 + 2 * self.nxt('pp', 2)]
                        pbk = PB[2] if pa is PB[1] else PB[4]
                        for kc in range(8):
                            MM(P, pa, pa.t[:], w.t[:, kc, cc * 128:(cc + 1) * 128], hm.t[:, kc, :], [w, hm], kc == 0, kc == 7)
                        for kc in range(8):
                            MM(P, pbk, pbk.t[:], w.t[:, kc, 256 + cc * 128:256 + (cc + 1) * 128], hm.t[:, kc, :], [w, hm],
                               kc == 0, kc == 7)
                        if b < 4:
                            t1, t2 = tmp[0], tmp[1]
                            TT(P, 'dve', t1, t1.t[:], pa.t[:], cosT.t[:], ALU.mult, [pa, cosT])
                            TT(P, 'dve', t2, t2.t[:], pbk.t[:], sinS.t[:], ALU.mult, [pbk, sinS])
                            if b % 2 == 0:
                                dst, dap = qT, qT.t[:, (b // 2) * 2 + cc, :]
                            else:
                                dst, dap = kT, kT.t[:, (b // 2) * 2 + cc, t0:t0 + 512]
                            TT(P, 'pool', dst, dap, t1.t[:], t2.t[:], ALU.add, [t1, t2])
                        else:
                            t1 = tmp[2]
                            ACT(P, t1, t1.t[:], pbk.t[:], AF.Sigmoid, [pbk])
                            TT(P, 'dve', glu, glu.t[:, cc, 30:542], pa.t[:], t1.t[:], ALU.mult, [pa, t1])
                else:
                    for sub in range(4):
                        pc = PB[5 + self.nxt('pc', 2)]
                        for kc in range(8):
                            MM(P, pc, pc.t[:], hm.t[:, kc, sub * 128:(sub + 1) * 128], w.t[:, kc, :], [w, hm], kc == 0, kc == 7)
                        if b == 5:
                            ACT(P, vaug, vaug.t[:, tt * 4 + sub, :, 0:64], pc.t[:].rearrange("p (h d) -> p h d", h=8),
                                AF.Copy, [pc])
                        else:
                            self.cpath(l, pc, sub, mixT, gl, vgn, wsTm, bb, stt, gnt, ocb, tmp)
            P.dma_cast(wreg.t[:], self.w_out[l].rearrange("(kc p) n -> p kc n", p=128), writes=[wblk[0], wblk[1]])
            if self.dbg_on and tt == 0:
                self.tap("q", qT, qT.t[:], [128, 4, 512], BF16)
                self.tap("k", kT, kT.t[:, :, 0:512], [128, 4, 512], BF16)
                self.tap("glu", glu, glu.t[:], [128, 2, 542])
                self.tap("v", vaug, vaug.t[:, 0:4, :, :], [128, 4, 8, 65], BF16)
            self.gating(tt, qT, kT, kms, kmb, gm, top8, sel, bq, biasT)
            dp = self.dpath(l, glu, acc, ybf, rstd, tmp, mixT)
            if self.dbg_on and tt == 1:
                self.tap("biasT", biasT, biasT.t[:], [8, 4, 512], BF16)
            for G in range(2):
                self.attn_group(G, tt, qT, kT, vaug, biasT, PT, og, rec, dp)
                if self.dbg_on and tt == 1:
                    self.tap("og%d" % G, og, og.t[:], [128, 4, 256])
                for qs in range(4):
                    self.group_norm_tm(l, og, og.t[:, qs, :], G * 2, mixT, qs * 128, gnt)
            for _ in dp:
                pass
            if self.dbg_on and tt == 1:
                self.tap("mixT", mixT, mixT.t[:], [128, 8, 512], BF16)
            for hf in range(2):
                c0 = hf * 256

                def ysrc(oc):
                    return PB[1 + oc // 2], PB[1 + oc // 2].t[:, (oc % 2) * 256:(oc % 2) * 256 + 256]
                for oc in range(8):
                    yb, yap = ysrc(oc)
                    for kc in range(8):
                        MM(P, yb, yap, wreg.t[:, kc, oc * 128:(oc + 1) * 128], mixT.t[:, kc, c0:c0 + 256],
                           [wblk[0], wblk[1], mixT], kc == 0, kc == 7)
                self.postnorm_residual(s, l, 0, t0 + c0, ysrc, 256, sqb, rstd, tmp)
        P.barrier()

    def cpath(self, l, pc, sub, mixT, gl, vgn, wsTm, bb, stt, gn_tmps, ocb, tmp):
        P = self.P
        tA, tB = tmp[2], tmp[3]
        ACT(P, tA, tA.t[:], pc.t[:], AF.Square, [pc])
        TS(P, 'pool', tA, tA.t[:], tA.t[:], 0.044715, 1.0, ALU.mult, ALU.add, [tA])
        TT(P, 'dve', tA, tA.t[:], tA.t[:], pc.t[:], ALU.mult, [tA, pc])
        ACT(P, tB, tB.t[:], tA.t[:], AF.Sigmoid, [tA], scale=1.5957691216057308)
        TT(P, 'dve', gl, gl.t[:], pc.t[:], tB.t[:], ALU.mult, [pc, tB])
        P.op('dve', lambda e: e.bn_stats(stt.t[:, 0:6], gl.t[:, 256:512]), [gl], [stt])
        P.op('dve', lambda e: e.bn_aggr(stt.t[:, 6:8], stt.t[:, 0:6]), [stt], [stt])
        TS(P, 'dve', stt, stt.t[:, 7:8], stt.t[:, 7:8], EPS, None, ALU.add, None, [stt])
        self.rsq(stt, stt.t[:, 7:8])
        TS(P, 'dve', vgn, vgn.t[:], gl.t[:, 256:512], stt.t[:, 6:7], stt.t[:, 7:8], ALU.subtract, ALU.mult, [gl, stt])
        pz = self.PB[7]
        for g in range(4):
            MM(P, pz, pz.t[:, 256 + g * 64:256 + (g + 1) * 64], wsTm.t[:, g, :], vgn.t[:, g * 64:(g + 1) * 64], [wsTm, vgn])
        TT(P, 'dve', ocb, ocb.t[:], pz.t[:, 256:512], bb.t[:], ALU.add, [pz, bb])
        TT(P, 'dve', ocb, ocb.t[:], ocb.t[:], gl.t[:, 0:256], ALU.mult, [ocb, gl])
        self.group_norm_tm(l, ocb, ocb.t[:], 4, mixT, sub * 128, gn_tmps)

    def dpath(self, l, glu, acc, ybf, rstd, tmp, mixT):
        P = self.P
        cf = self.cf
        PB = self.PB
        for cc in range(2):
            eng = 'dve'
            a = acc[cc]
            wbase = (l * 2 + cc) * 31
            TS(P, eng, a, a.t[:], glu.t[:, cc, 0:512], self.c('convw', wbase, 1), self.c('convv', (l * 3 + 0) * 2 + cc, 1),
               ALU.mult, ALU.add, [glu, cf])
            for j in range(1, 31):
                STT(P, eng, a, a.t[:], glu.t[:, cc, j:j + 512], self.c('convw', wbase + j, 1), a.t[:], ALU.mult, ALU.add,
                    [glu, cf, a])
                if j % 8 == 0:
                    yield
        for cc in range(2):
            CP(P, 'dve', glu, glu.t[:, cc, 0:30], glu.t[:, cc, 512:542], [glu])
        p1, p2 = PB[5], PB[6]
        for cc in range(2):
            ACT(P, ybf, ybf.t[:, cc, :], acc[cc].t[:], AF.Copy, [acc[cc]])
            MM(P, p1, p1.t[:], self.ones.t[:], ybf.t[:, cc, :], [ybf, self.ones], cc == 0, cc == 1)
        for cc in range(2):
            ACT(P, ybf, ybf.t[:, cc, :], acc[cc].t[:], AF.Square, [acc[cc]])
            MM(P, p2, p2.t[:], self.ones.t[:], ybf.t[:, cc, :], [ybf, self.ones], cc == 0, cc == 1)
        t1, mean = tmp[0], tmp[1]
        TS(P, 'dve', mean, mean.t[:], p1.t[:], 1.0 / 256, None, ALU.mult, None, [p1])
        TT(P, 'dve', t1, t1.t[:], mean.t[:], mean.t[:], ALU.mult, [mean])
        STT(P, 'dve', t1, t1.t[:], p2.t[:], 1.0 / 256, t1.t[:], ALU.mult, ALU.subtract, [p2, t1])
        TS(P, 'dve', rstd, rstd.t[:], t1.t[:], EPS, None, ALU.add, None, [t1])
        self.rsq(rstd, rstd.t[:])
        for cc in range(2):
            a = acc[cc]
            TT(P, 'dve', a, a.t[:], a.t[:], mean.t[:], ALU.subtract, [a, mean])
            TT(P, 'dve', a, a.t[:], a.t[:], rstd.t[:], ALU.mult, [a, rstd])
            ACT(P, a, a.t[:], a.t[:], AF.Silu, [a, cf], bias=self.c('convv', (l * 3 + 2) * 2 + cc, 1),
                scale=self.c('convv', (l * 3 + 1) * 2 + cc, 1))
        for cc in range(2):
            ACT(P, ybf, ybf.t[:, cc, :], acc[cc].t[:], AF.Square, [acc[cc]])
            MM(P, p1, p1.t[:], self.ones.t[:], ybf.t[:, cc, :], [ybf, self.ones], cc == 0, cc == 1)
        TS(P, 'dve', rstd, rstd.t[:], p1.t[:], 1.0 / 256, EPS, ALU.mult, ALU.add, [p1])
        self.rsq(rstd, rstd.t[:])
        for cc in range(2):
            a = acc[cc]
            TT(P, 'dve', a, a.t[:], a.t[:], rstd.t[:], ALU.mult, [a, rstd])
            ACT(P, mixT, mixT.t[:, 6 + cc, :], a.t[:], AF.Identity, [a, cf], scale=self.c('ggain', l * 8 + 6 + cc, 1))

    def gating(self, tt, qT, kT, kms, kmb, gm, top8, sel, bq, biasT):
        P = self.P
        cf = self.cf
        for cc in range(2):
            for j in (2 * tt, 2 * tt + 1):
                P.op('dve', lambda e, cc=cc, j=j: e.tensor_reduce(kms.t[:, cc, j:j + 1], kT.t[:, cc, j * 256:(j + 1) * 256],
                                                                  AX.X, ALU.add), [kT], [kms])
        TS(P, 'dve', kmb, kmb.t[:], kms.t[:], 1.0 / 256, None, ALU.mult, None, [kms])
        pg = self.PB[7]
        ptv = pg.t[:].bitcast(BF16)
        for sub in range(4):
            nq = (tt * 4 + sub) // 2
            for h in range(4):
                pb0 = 64 * (h % 2)
                MM(P, pg, pg.t[:, h * 8:(h + 1) * 8], qT.t[pb0:pb0 + 64, h // 2, sub * 128:(sub + 1) * 128],
                   kmb.t[pb0:pb0 + 64, h // 2, :], [qT, kmb])
            TT(P, 'dve', gm, gm.t[:], pg.t[:, 0:32], self.c('pastneg', nq * 32, 32), ALU.add, [pg, cf])
            for h in range(4):
                P.op('dve', lambda e, h=h: e.max(top8.t[:, h, :], gm.t[:, h * 8:(h + 1) * 8]), [gm], [top8])
            for h in range(4):
                TS(P, 'dve', sel, sel.t[:, h * 8:(h + 1) * 8], gm.t[:, h * 8:(h + 1) * 8], top8.t[:, h, 2:3], None, ALU.is_ge,
                   None, [gm, top8])
            TT(P, 'dve', sel, sel.t[:], sel.t[:], self.c('past', nq * 32, 32), ALU.mult, [sel, cf])
            TT(P, 'dve', sel, sel.t[:], sel.t[:], self.c('own', nq * 32, 32), ALU.add, [sel, cf])
            TS(P, 'dve', bq, bq.t[:], sel.t[:], BIG, -BIG, ALU.mult, ALU.add, [sel])
            for h in range(4):
                TR(P, pg, ptv[0:8, 128 + h * 128:128 + (h + 1) * 128], bq.t[:, h * 8:(h + 1) * 8], self.ident.t[:],
                   [bq, self.ident])
            CP(P, 'dve', biasT, biasT.t[:, :, sub * 128:(sub + 1) * 128],
               ptv[0:8, 128:640].rearrange("p (h q) -> p h q", h=4), [pg])

    def attn_group(self, G, qt, qT, kT, vaug, biasT, PT, ogb, rec, dp):
        P = self.P
        PB = self.PB
        nk = 4 * qt + 4
        steps = [(h, kt) for h in range(4) for kt in range(nk)]
        n = len(steps)
        LA = 3
        Sbanks = [PB[1], PB[2], PB[0], PB[7]]
        Es = {}

        def emit_S(i):
            h, kt = steps[i]
            cq = G * 2 + h // 2
            pb0 = 64 * (h % 2)
            Sb = Sbanks[i % 4]
            MM(P, Sb, Sb.t[:], kT.t[pb0:pb0 + 64, cq, kt * 128:(kt + 1) * 128], qT.t[pb0:pb0 + 64, cq, :], [kT, qT],
               True, G == 1)
            if G == 0:
                jb = kt // 2
                MM(P, Sb, Sb.t[:], self.indc.t[0:8, jb * 128:(jb + 1) * 128], biasT.t[0:8, h, :], [self.indc, biasT],
                   False, True)
            E = PT[i % 5]
            ACT(P, E, E.t[:], Sb.t[:], AF.Exp, [Sb], scale=0.125)
            dl = 512 * qt - 128 * kt
            if G == 1:
                mi = (dl + 384) // 128 if dl <= 512 else 8
                TT(P, 'dve', E, E.t[:], E.t[:], self.mB.t[:, mi, :], ALU.mult, [E, self.mB])
            elif dl <= 0:
                TT(P, 'dve', E, E.t[:], E.t[:], self.mA.t[:, (-dl) // 128, :], ALU.mult, [E, self.mA])
            Es[i] = E

        def emit_PV(i):
            h, kt = steps[i]
            O = PB[3 + h % 2]
            E = Es.pop(i)
            for qs in range(4):
                last = 4 * qt + qs
                if kt <= last:
                    MM(P, O, O.t[:, qs * 65:(qs + 1) * 65], E.t[:, qs * 128:(qs + 1) * 128], vaug.t[:, kt, G * 4 + h, :],
                       [E, vaug], kt == 0 and qs == 0, kt == last)
            if kt == nk - 1:
                O3 = O.t[:, 0:260].rearrange("p (q d) -> p q d", q=4)
                P.op('dve', lambda e: e.reciprocal(rec.t[:].rearrange("p (q o) -> p q o", o=1), O3[:, :, 64:65]), [O], [rec])
                for qs in range(4):
                    TS(P, 'dve', ogb, ogb.t[:, qs, h * 64:(h + 1) * 64], O.t[:, qs * 65:qs * 65 + 64], rec.t[:, qs:qs + 1],
                       None, ALU.mult, None, [O, rec])
                next(dp, None)

        for i in range(min(LA, n)):
            emit_S(i)
        for i in range(n):
            if i + LA < n:
                emit_S(i + LA)
            emit_PV(i)

    def ffn(self, s, l, last=False):
        P = self.P
        ar = self.ar
        PB = self.PB
        moe = (l == 1)
        nfb = (DFE if moe else DFF) // 256
        for grp in range(2):
            g0 = grp * 1024
            ar.reset()
            cv = ar.carve
            hT = cv("hT", [8, 1024], BF16)
            yacc = cv("yacc", [8, 1024], F32)
            act = [cv("act%d" % i, [2, 1024], BF16) for i in range(2)]
            wg = [cv("wg%d" % i, [8, 256], BF16) for i in range(2)]
            wu = [cv("wu%d" % i, [8, 256], BF16) for i in range(2)]
            wd = [cv("wd%d" % i, [2, 1024], BF16) for i in range(2)]
            sqb = [cv("sq%d" % i, [512], BF16) for i in range(2)]
            rstd = cv("rstd", [512], F32)
            tmp = [cv("tmp%d" % i, [512], F32) for i in range(4)]
            for tl in range(2):
                self.prenorm(s, l, 1, g0 + tl * 512, hT, tl * 512, sqb, rstd, tmp)
            if (not moe) and s == 0 and grp == 0 and self.mod_deferred:
                wbd = [cv("wadad%d" % i, [8, 512], BF16) for i in range(2)]
                modgen = self.mod_layer_gen(1, wbd, 512)
            else:
                modgen = iter(())
            if moe:
                gatesT = cv("gatesT", [1024], F32, parts=8)
                indf = cv("indf", [1024], F32, parts=8)
                gbc = cv("gbc", [2, 512], F32)
                lg = cv("lg", [8], F32)
                top8 = cv("top8", [8], F32)
                sm = cv("sm", [8], F32)
                e1 = cv("e1", [8], F32)
                e2 = cv("e2", [8], F32)
                P.dma(indf.t[:], self.indc_d, writes=[indf])
                self.router(hT, gatesT, lg, top8, sm, e1, e2)
                blocks = [(e, fb) for e in range(NE) for fb in range(nfb)]
            else:
                blocks = [(0, fb) for fb in range(nfb)]
            nb = len(blocks)

            def wsrcs(i):
                e_, fb_ = blocks[i]
                if moe:
                    return self.moe_wg[e_], self.moe_wu[e_], self.moe_wd[e_], fb_ * 256
                return self.ffn_wg, self.ffn_wu, self.ffn_wd, fb_ * 256

            def issue_gu(i):
                if i >= nb:
                    return
                sg_, su_, sd_, c0 = wsrcs(i)
                P.dma_cast(wg[i % 2].t[:], sg_[:, c0:c0 + 256].rearrange("(kc p) n -> p kc n", p=128), writes=[wg[i % 2]])
                P.dma_cast(wu[i % 2].t[:], su_[:, c0:c0 + 256].rearrange("(kc p) n -> p kc n", p=128), writes=[wu[i % 2]])

            def issue_d(i):
                if i >= nb:
                    return
                sg_, su_, sd_, c0 = wsrcs(i)
                P.dma_cast(wd[i % 2].t[:], sd_[c0:c0 + 256, :].rearrange("(fc p) n -> p fc n", p=128), writes=[wd[i % 2]])

            def gu(bi):
                e, fb = blocks[bi]
                wgb, wub, ab = wg[bi % 2], wu[bi % 2], act[bi % 2]
                if moe and fb == 0:
                    for tl in range(2):
                        pgb = PB[0]
                        MM(P, pgb, pgb.t[:], indf.t[0:8, e * 128:(e + 1) * 128], gatesT.t[0:8, tl * 512:(tl + 1) * 512],
                           [indf, gatesT])
                        ACT(P, gbc, gbc.t[:, tl, :], pgb.t[:], AF.Copy, [pgb])
                for fc in range(2):
                    for tl in range(2):
                        pg = PB[1 + 2 * self.nxt('fg', 2)]
                        pu = PB[2] if pg is PB[1] else PB[4]
                        for kc in range(8):
                            MM(P, pg, pg.t[:], wgb.t[:, kc, fc * 128:(fc + 1) * 128], hT.t[:, kc, tl * 512:(tl + 1) * 512],
                               [wgb, hT], kc == 0, kc == 7)
                        for kc in range(8):
                            MM(P, pu, pu.t[:], wub.t[:, kc, fc * 128:(fc + 1) * 128], hT.t[:, kc, tl * 512:(tl + 1) * 512],
                               [wub, hT], kc == 0, kc == 7)
                        t1 = tmp[self.nxt('ft', 2)]
                        ACT(P, t1, t1.t[:], pg.t[:], AF.Silu, [pg])
                        if moe:
                            TT(P, 'dve', t1, t1.t[:], t1.t[:], gbc.t[:, tl, :], ALU.mult, [t1, gbc])
                        TT(P, 'dve', ab, ab.t[:, fc, tl * 512:(tl + 1) * 512], t1.t[:], pu.t[:], ALU.mult, [t1, pu])

            def down(bi):
                wdb, ab = wd[bi % 2], act[bi % 2]
                for oc in range(8):
                    for tl in range(2):
                        py = PB[5 + self.nxt('fy', 3)]
                        for fc in range(2):
                            MM(P, py, py.t[:], wdb.t[:, fc, oc * 128:(oc + 1) * 128], ab.t[:, fc, tl * 512:(tl + 1) * 512],
                               [wdb, ab], fc == 0, fc == 1)
                        ya = yacc.t[:, oc, tl * 512:(tl + 1) * 512]
                        if bi == 0:
                            ACT(P, yacc, ya, py.t[:], AF.Copy, [py])
                        else:
                            TT(P, 'dve', yacc, ya, ya, py.t[:], ALU.add, [yacc, py])

            issue_gu(0)
            issue_d(0)
            issue_gu(1)
            issue_d(1)
            gu(0)
            for bi in range(nb):
                if bi + 1 < nb:
                    gu(bi + 1)
                issue_gu(bi + 2)
                down(bi)
                issue_d(bi + 2)
                next(modgen, None)
            for _ in modgen:
                pass
            for tl in range(2):
                self.postnorm_residual(s, l, 1, g0 + tl * 512, lambda c, tl=tl: (yacc, yacc.t[:, c, tl * 512:(tl + 1) * 512]),
                                       512, sqb, rstd, tmp)
            P.barrier()
            if last:
                for tile in (2 * grp, 2 * grp + 1):
                    self.xstore(s, tile)
                    if s + 1 < NSEQ:
                        self.xload(s + 1, tile)

    def router(self, hT, gatesT, lg, top8, sm, e1, e2):
        P = self.P
        pl = self.PB[7]
        for sub in range(8):
            for kc in range(8):
                MM(P, pl, pl.t[:, 0:8], hT.t[:, kc, sub * 128:(sub + 1) * 128], self.rwb.t[:, kc, :], [hT, self.rwb],
                   kc == 0, kc == 7)
            TT(P, 'dve', lg, lg.t[:], pl.t[:, 0:8], self.rb.t[:], ALU.add, [pl, self.rb])
            P.op('dve', lambda e: e.max(top8.t[:], lg.t[:]), [lg], [top8])
            TS(P, 'dve', sm, sm.t[:, 0:1], top8.t[:, 1:2], top8.t[:, 0:1], None, ALU.subtract, None, [top8])
            ACT(P, sm, sm.t[:, 1:2], sm.t[:, 0:1], AF.Exp, [sm])
            TS(P, 'dve', sm, sm.t[:, 2:3], sm.t[:, 1:2], 1.0, None, ALU.add, None, [sm])
            P.op('dve', lambda e: e.reciprocal(sm.t[:, 3:4], sm.t[:, 2:3]), [sm], [sm])
            TT(P, 'dve', sm, sm.t[:, 4:5], sm.t[:, 1:2], sm.t[:, 3:4], ALU.mult, [sm])
            TS(P, 'dve', e1, e1.t[:], lg.t[:], top8.t[:, 0:1], sm.t[:, 3:4], ALU.is_equal, ALU.mult, [lg, top8, sm])
            TS(P, 'dve', e2, e2.t[:], lg.t[:], top8.t[:, 1:2], sm.t[:, 4:5], ALU.is_equal, ALU.mult, [lg, top8, sm])
            TT(P, 'dve', e1, e1.t[:], e1.t[:], e2.t[:], ALU.add, [e1, e2])
            TR(P, pl, pl.t[0:8, 128:256], e1.t[:], self.identf.t[:], [e1, self.identf])
            CP(P, 'dve', gatesT, gatesT.t[:, sub * 128:(sub + 1) * 128], pl.t[0:8, 128:256], [pl])

    def xload(self, s, tile):
        a, b = tile * 512, (tile + 1) * 512
        self.P.dma(self.xT.t[:, :, a:b], self.xin[s].rearrange("(c p) t -> p c t", p=128)[:, :, a:b], writes=[self.xTt[tile]])

    def xstore(self, s, tile):
        a, b = tile * 512, (tile + 1) * 512
        self.P.dma(self.xout[s].rearrange("(c p) t -> p c t", p=128)[:, :, a:b], self.xT.t[:, :, a:b], reads=[self.xTt[tile]])

    def build(self):
        P = self.P
        self.setup()
        self.early_io = (self.phases[-1][0] == 'ffn')
        for tile in range(4):
            self.xload(0, tile)
        for s in range(NSEQ):
            if s > 0 and not self.early_io:
                for tile in range(4):
                    self.xload(s, tile)
            for pi, ph in enumerate(self.phases):
                if ph[0] == 'mix':
                    self.mixer(s, ph[1])
                else:
                    self.ffn(s, ph[1], last=(self.early_io and pi == len(self.phases) - 1))
            if not self.early_io:
                for tile in range(4):
                    self.xstore(s, tile)
                P.barrier()
        P.finish()
        return self.nc


def _consts():
    p = np.arange(128)[:, None]
    j = np.arange(512)[None, :]

    def wB(d):
        d = d.astype(np.int64)
        return (((d >= 0) & (d <= 128)).astype(np.float32) + ((d >= 0) & (d % 4 == 0) & (d <= 512)).astype(np.float32)
                + ((d >= 0) & (d % 16 == 0) & (d <= 2048)).astype(np.float32))

    mB = np.zeros((128, 9, 512), np.float32)
    for i in range(8):
        dl = -384 + 128 * i
        mB[:, i, :] = wB(dl + j - p)
    mB[:, 8, :] = ((j - p) % 16 == 0).astype(np.float32)
    mA = np.zeros((128, 4, 512), np.float32)
    for i in range(4):
        mA[:, i, :] = ((-128 * i + j - p) >= 0).astype(np.float32)
    tri = (np.arange(128)[:, None] <= np.arange(128)[None, :]).astype(np.float32)
    indc = np.zeros((8, 8, 128), np.float32)
    for jb in range(8):
        indc[jb, jb, :] = 1.0
    return mB.reshape(128, -1), mA.reshape(128, -1), tri, indc.reshape(8, 1024)


def _prep_shared(inp):
    f = lambda a: np.ascontiguousarray(np.asarray(a, dtype=np.float32))
    sh = {}
    mB, mA, tri, indc = _consts()
    sh.update(mB=mB, mA=mA, tri=tri, indc=indc)
    sh['ada_w'] = f(inp['ada_w'])
    w_in = np.asarray(inp['w_in'], np.float32)
    sl = lambda i: np.arange(i * 256, (i + 1) * 256)
    swp = np.concatenate([np.concatenate([np.arange(h * 64 + 32, h * 64 + 64), np.arange(h * 64, h * 64 + 32)]) for h in range(4)])
    qa, ka, va, qb, kb, vb, u, vg, ga, gg = [sl(i) for i in range(10)]
    cols = np.concatenate([qa, qa[swp], ka, ka[swp], qb, qb[swp], kb, kb[swp], ga, gg, va, vb, u, vg])
    sh['w_in'] = np.ascontiguousarray(w_in[:, :, cols])
    sh['w_out'] = f(inp['w_out'])
    sh['wsT'] = np.ascontiguousarray(np.asarray(inp['gmlp_ws'], np.float32).transpose(0, 1, 3, 2))
    sh['ffn_wg'] = f(inp['ffn_w_gate'][0])
    sh['ffn_wu'] = f(inp['ffn_w_up'][0])
    sh['ffn_wd'] = f(inp['ffn_w_down'][0])
    sh['moe_wg'] = f(inp['moe_w_gate'][0])
    sh['moe_wu'] = f(inp['moe_w_up'][0])
    sh['moe_wd'] = f(inp['moe_w_down'][0])
    sh['rbrep'] = np.ascontiguousarray(np.broadcast_to(np.asarray(inp['router_b'], np.float32)[0][None, :], (128, 8)))
    cf = np.zeros((128, NCF), np.float32)

    def put(name, arr):
        off, n = CM[name]
        cf[:, off:off + n] = np.asarray(arr, np.float32).reshape(128, n)

    half = 32
    invf = np.power(np.float32(10000.0), -np.arange(half, dtype=np.float32) / np.float32(half)).astype(np.float32)
    pidx = np.arange(128)
    put('invf', (invf[pidx % 32].astype(np.float64) / (2 * math.pi)).astype(np.float32))
    sgn = np.where((pidx % 64) < 32, -1.0, 1.0).astype(np.float32)
    put('sgn', (6.28318 * sgn).astype(np.float32))
    put('npsgn', (-math.pi * sgn).astype(np.float32))
    put('negpi', np.full(128, -math.pi, np.float32))
    gains = np.stack([np.asarray(inp[k], np.float32) for k in ('mix_pre_g', 'mix_post_g', 'ffn_pre_g', 'ffn_post_g')], 1)
    put('gains', gains.reshape(2, 4, 8, 128).transpose(3, 0, 1, 2))
    put('ggain', np.asarray(inp['group_out_g'], np.float32).reshape(2, 8, 128).transpose(2, 0, 1))
    put('gb', np.asarray(inp['gmlp_b'], np.float32).transpose(2, 0, 1))
    put('convw', np.asarray(inp['conv_w'], np.float32).reshape(2, 31, 2, 128).transpose(3, 0, 2, 1))
    cvv = np.stack([np.asarray(inp[k], np.float32) for k in ('conv_b', 'conv_ln_g', 'conv_ln_b')], 1)
    put('convv', cvv.reshape(2, 3, 2, 128).transpose(3, 0, 1, 2))
    put('ada_b', np.asarray(inp['ada_b'], np.float32).reshape(2, 48, 128).transpose(2, 0, 1))
    put('rw', np.asarray(inp['router_w'], np.float32)[0].reshape(8, 128, 8).transpose(1, 0, 2))
    n = np.arange(8)[:, None, None]
    jj = np.arange(8)[None, None, :]
    past = np.broadcast_to((jj < n), (8, 4, 8)).astype(np.float32)
    own = np.broadcast_to((jj == n), (8, 4, 8)).astype(np.float32)
    put('past', np.broadcast_to(past.reshape(1, -1), (128, 256)))
    put('own', np.broadcast_to(own.reshape(1, -1), (128, 256)))
    put('pastneg', np.broadcast_to(((1.0 - past) * -1e30).reshape(1, -1), (128, 256)))
    put('ones', np.ones((128, 64), np.float32))
    sh['_cf'] = cf
    return sh


def _core_inputs(inp, sh, core):
    b0 = core * NSEQ
    m = {k: v for k, v in sh.items() if not k.startswith('_')}
    x = np.asarray(inp['x'], np.float32)[b0:b0 + NSEQ]
    m['xT'] = np.ascontiguousarray(x.transpose(0, 2, 1))
    pos = np.asarray(inp['positions'], np.int32)[b0:b0 + NSEQ]
    m['posrep'] = np.ascontiguousarray(np.broadcast_to(pos[:, None, :], (NSEQ, 128, S)))
    cf = sh['_cf'].copy()
    off, n = CM['cT']
    c = np.asarray(inp['c'], np.float32)[b0:b0 + NSEQ]
    cf[:, off:off + n] = c.reshape(NSEQ, 8, 128).transpose(2, 1, 0).reshape(128, n)
    m['cf'] = cf
    return m


ALL_PHASES = [('mix', 0), ('ffn', 0), ('mix', 1), ('ffn', 1)]


def kernel(**inputs):
    sh = _prep_shared(inputs)
    nc = K(ALL_PHASES).build()
    in_maps = [_core_inputs(inputs, sh, c) for c in range(NCORES)]
    res = run_bass_kernel_spmd(nc, in_maps, core_ids=list(range(NCORES)))
    out = np.concatenate([np.asarray(r["outT"], np.float32).transpose(0, 2, 1) for r in res.results], axis=0)
    return np.ascontiguousarray(out.astype(np.float32))
```

```python
from contextlib import ExitStack
import math
import numpy as np
import concourse.bass as bass
import concourse.mybir as mybir
from concourse.bass_utils import run_bass_kernel_spmd

F32 = mybir.dt.float32
BF16 = mybir.dt.bfloat16
I32 = mybir.dt.int32
AF = mybir.ActivationFunctionType
ALU = mybir.AluOpType
AX = mybir.AxisListType

S = 2048
D = 1024
NCORES = 8
NSEQ = 2
DFF = 2816
DFE = 3584
NE = 8
EPS = 1e-6
BIG = 30000.0
TWO_PI = 2.0 * math.pi

ENGS = ('pe', 'act', 'dve', 'pool', 'sp')
N_DMA_SEMS = 40
N_SP_SEMS = 8


class Buf:
    def __init__(self, name, t=None):
        self.name = name
        self.t = t
        self.last_w = None
        self.readers = {}


class _Op:
    __slots__ = ('eng', 'fn', 'deps', 'idx', 'needed', 'dma', 'sem', 'val', 'cnt')

    def __init__(self, eng, fn, dma):
        self.eng = eng
        self.fn = fn
        self.deps = []
        self.needed = False
        self.dma = dma
        self.sem = None
        self.val = 0
        self.cnt = 0


class Prog:
    def __init__(self, nc):
        self.nc = nc
        self.stack = ExitStack()
        self.ops = {e: [] for e in ENGS}
        self.n_dma = 0
        self.n_dma_sp = 0
        self.dma_last = [None] * N_DMA_SEMS
        self.dma_uses = [0] * N_DMA_SEMS
        self.pending = {e: [] for e in ENGS}

    def sb(self, name, shape, dt):
        t = self.stack.enter_context(self.nc.sbuf_tensor(name, list(shape), dt))
        return Buf(name, t)

    def ps(self, name, shape, dt):
        t = self.stack.enter_context(self.nc.psum_tensor(name, list(shape), dt))
        return Buf(name, t)

    def _add(self, eng, fn, reads, writes, dma=False):
        op = _Op(eng, fn, dma)
        op.idx = len(self.ops[eng])
        deps = list(self.pending[eng])
        self.pending[eng] = []
        for b in reads:
            if b.last_w is not None:
                deps.append(b.last_w)
        for b in writes:
            if b.last_w is not None:
                deps.append(b.last_w)
            deps.extend(b.readers.values())
        for b in reads:
            b.readers[('dma', id(op)) if dma else eng] = op
        for b in writes:
            b.last_w = op
            b.readers = {}
        seen = set()
        for d in deps:
            if d is op or id(d) in seen:
                continue
            seen.add(id(d))
            if (not d.dma) and (not dma) and d.eng == eng == 'pe':
                continue
            op.deps.append(d)
            d.needed = True
        self.ops[eng].append(op)
        return op

    def op(self, eng, fn, reads=(), writes=()):
        return self._add(eng, fn, reads, writes)

    def _dma(self, eng, fn, reads, writes):
        op = self._add(eng, fn, reads, writes, dma=True)
        if eng == 'sp':
            k = self.n_dma_sp % N_SP_SEMS
            self.n_dma_sp += 1
        else:
            k = N_SP_SEMS + self.n_dma % (N_DMA_SEMS - N_SP_SEMS)
            self.n_dma += 1
        prev = self.dma_last[k]
        if prev is not None:
            op.deps.append(prev)
        self.dma_uses[k] += 1
        op.sem = k
        op.val = 16 * self.dma_uses[k]
        self.dma_last[k] = op
        return op

    def dma(self, out, in_, reads=(), writes=()):
        return self._dma('sp', lambda e: e.dma_start(out=out, in_=in_), reads, writes)

    def dma_cast(self, out, in_, reads=(), writes=()):
        return self._dma('pool', lambda e: e.dma_start(out=out, in_=in_), reads, writes)

    def barrier(self):
        lasts = [o for o in self.dma_last if o is not None]
        for e in ENGS:
            for o in reversed(self.ops[e]):
                if not o.dma:
                    o.needed = True
                    lasts.append(o)
                    break
        for e in ENGS:
            self.pending[e] = list(lasts)

    def finish(self):
        nc = self.nc
        self.barrier()
        fin = _Op('sp', None, False)
        fin.deps = list(self.pending['sp'])
        self.ops['sp'].append(fin)
        for e in ENGS:
            c = 0
            for o in self.ops[e]:
                if (not o.dma) and o.needed:
                    c += 1
                    o.cnt = c
        st = self.stack
        esem = {e: st.enter_context(nc.semaphore("s_" + e)) for e in ENGS}
        dsem = [st.enter_context(nc.semaphore("d%d" % k)) for k in range(N_DMA_SEMS)]
        block = st.enter_context(nc.Block())
        ops = self.ops

        def run(ename, eng):
            waited = {}
            for o in ops[ename]:
                need = {}
                for d in o.deps:
                    if d.dma:
                        key = ('d', d.sem)
                        v = d.val
                    else:
                        key = ('e', d.eng)
                        v = d.cnt
                    if v > need.get(key, 0):
                        need[key] = v
                for key, v in need.items():
                    if waited.get(key, 0) >= v:
                        continue
                    waited[key] = v
                    eng.wait_ge(dsem[key[1]] if key[0] == 'd' else esem[key[1]], v)
                if o.fn is None:
                    continue
                ins = o.fn(eng)
                if o.dma:
                    ins.then_inc(dsem[o.sem], 16)
                elif o.needed:
                    ins.then_inc(esem[ename], 1)

        @block.tensor
        def _(eng):
            run('pe', eng)

        @block.scalar
        def _(eng):
            run('act', eng)

        @block.vector
        def _(eng):
            run('dve', eng)

        @block.gpsimd
        def _(eng):
            run('pool', eng)

        @block.sync
        def _(eng):
            run('sp', eng)

        st.close()


_DTSIZE = {F32: 4, BF16: 2, I32: 4}


class Arena:
    def __init__(self, P, name, nwords):
        self.t = P.sb(name, [128, nwords], F32).t
        self.cap = nwords
        self.off = 0

    def reset(self):
        self.off = 0

    def carve(self, name, shape, dt, parts=128):
        n = 1
        for s_ in shape:
            n *= s_
        words = (n * _DTSIZE[dt] + 3) // 4
        assert self.off + words <= self.cap, (name, self.off, words, self.cap)
        ap = self.t[0:parts, self.off:self.off + words]
        if dt != F32:
            ap = ap.bitcast(dt)
        if len(shape) == 2:
            ap = ap.rearrange("p (a b) -> p a b", a=shape[0])
        elif len(shape) == 3:
            ap = ap.rearrange("p (a b c) -> p a b c", a=shape[0], b=shape[1])
        self.off += words
        return Buf(name, ap)


def MM(P, ob, o, a, b, rd, st=True, sp=True):
    P.op('pe', lambda e: e.matmul(o, a, b, start=st, stop=sp), rd, [ob])


def TR(P, ob, o, a, ident, rd):
    P.op('pe', lambda e: e.transpose(o, a, ident), rd, [ob])


def ACT(P, ob, o, i, f, rd, bias=0.0, scale=1.0, acc=None, wr=None):
    if acc is None:
        P.op('act', lambda e: e.activation(o, i, f, bias=bias, scale=scale), rd, wr or [ob])
    else:
        P.op('act', lambda e: e.activation(o, i, f, bias=bias, scale=scale, accum_out=acc), rd, wr or [ob])


def TT(P, eng, ob, o, a, b, op, rd):
    P.op(eng, lambda e: e.tensor_tensor(o, a, b, op), rd, [ob])


def TS(P, eng, ob, o, a, s1, s2, op0, op1, rd):
    if op1 is None:
        P.op(eng, lambda e: e.tensor_scalar(o, a, s1, None, op0), rd, [ob])
    else:
        P.op(eng, lambda e: e.tensor_scalar(o, a, s1, s2, op0, op1), rd, [ob])


def STT(P, eng, ob, o, a, s, b, op0, op1, rd):
    P.op(eng, lambda e: e.scalar_tensor_tensor(o, a, s, b, op0, op1), rd, [ob])


def CP(P, eng, ob, o, i, rd):
    P.op(eng, lambda e: e.tensor_copy(o, i), rd, [ob])


def _cmap():
    m = {}
    off = 0
    for name, n in (('invf', 1), ('sgn', 1), ('npsgn', 1), ('negpi', 1), ('gains', 64), ('ggain', 16),
                    ('gb', 8), ('convw', 124), ('convv', 12), ('ada_b', 96), ('rw', 64),
                    ('pastneg', 256), ('past', 256), ('own', 256), ('cT', 8 * NSEQ), ('ones', 64)):
        m[name] = (off, n)
        off += n
    return m, off


CM, NCF = _cmap()


class K:
    def __init__(self, phases, dbg=False):
        self.phases = phases
        nc = bass.Bass("TRN2", target_bir_lowering=False)
        self.nc = nc
        P = Prog(nc)
        self.P = P

        def din(name, shape, dt=F32):
            return nc.dram_tensor(name, list(shape), dt, kind="ExternalInput").ap()

        self.xin = din("xT", [NSEQ, D, S])
        self.xout = nc.dram_tensor("outT", [NSEQ, D, S], F32, kind="ExternalOutput").ap()
        self.posrep = din("posrep", [NSEQ, 128, S], I32)
        self.cf_d = din("cf", [128, NCF])
        self.mB_d = din("mB", [128, 9 * 512])
        self.mA_d = din("mA", [128, 4 * 512])
        self.tri_d = din("tri", [128, 128])
        self.indc_d = din("indc", [8, 1024])
        self.rb_d = din("rbrep", [128, 8])
        self.ada_w = din("ada_w", [2, D, 6 * D])
        self.w_in = din("w_in", [2, D, 3584])
        self.w_out = din("w_out", [2, D, D])
        self.wsT = din("wsT", [2, 4, 128, 128])
        self.ffn_wg = din("ffn_wg", [D, DFF])
        self.ffn_wu = din("ffn_wu", [D, DFF])
        self.ffn_wd = din("ffn_wd", [DFF, D])
        self.moe_wg = din("moe_wg", [NE, D, DFE])
        self.moe_wu = din("moe_wu", [NE, D, DFE])
        self.moe_wd = din("moe_wd", [NE, DFE, D])
        self.dbg = {}
        self.dbg_on = dbg

        self.xT = P.sb("xTs", [128, 8, S], F32)
        self.xTt = [Buf("xTt%d" % i, self.xT.t) for i in range(4)]
        self.cf = P.sb("cfs", [128, NCF], F32)
        self.ident = P.sb("ident", [128, 128], BF16)
        self.identf = P.sb("identf", [128, 128], F32)
        self.ones = P.sb("ones", [128, 128], BF16)
        self.mB = P.sb("mBs", [128, 9, 512], BF16)
        self.mA = P.sb("mAs", [128, 4, 512], BF16)
        self.tri = P.sb("tris", [128, 128], BF16)
        self.indc = P.sb("indcs", [8, 1024], BF16)
        self.rb = P.sb("rbs", [128, 8], F32)
        self.cact = P.sb("cact", [128, 8, NSEQ], BF16)
        self.modv = P.sb("modv", [128, 2, 48, NSEQ], F32)
        self.drv = P.sb("drv", [128, 2, NSEQ, 4, 8], F32)
        self.rwb = P.sb("rwb", [128, 8, 8], BF16)
        self.PB = [P.ps("pb%d" % i, [128, 512], F32) for i in range(8)]
        self.ar = Arena(P, "arena", 30000)
        self.rot = {}

    def c(self, name, a=0, n=None):
        off, nn = CM[name]
        if n is None:
            n = nn - a
        return self.cf.t[:, off + a:off + a + n]

    def nxt(self, key, n):
        v = self.rot.get(key, 0)
        self.rot[key] = (v + 1) % n
        return v

    def tap(self, name, buf, ap, shape, dt=F32):
        if not self.dbg_on or name in self.dbg:
            return
        d = self.nc.dram_tensor("dbg_" + name, list(shape), dt, kind="ExternalOutput").ap()
        self.dbg[name] = d
        self.P.dma(d, ap, reads=[buf])

    def setup(self):
        P = self.P
        P.dma(self.cf.t[:], self.cf_d, writes=[self.cf])
        P.dma(self.rb.t[:], self.rb_d, writes=[self.rb])
        P.dma_cast(self.mB.t[:], self.mB_d.rearrange("p (a b) -> p a b", a=9), writes=[self.mB])
        P.dma_cast(self.mA.t[:], self.mA_d.rearrange("p (a b) -> p a b", a=4), writes=[self.mA])
        P.dma_cast(self.tri.t[:], self.tri_d, writes=[self.tri])
        P.dma_cast(self.indc.t[:], self.indc_d, writes=[self.indc])
        for idb, val in ((self.ident, 1.0), (self.identf, 1.0)):
            P.op('pool', lambda e, t=idb.t: e.memset(t[:], 1.0), (), [idb])
            P.op('pool', lambda e, t=idb.t: e.affine_select(t[:], t[:], [[-1, 128]], ALU.is_equal, 0.0, base=0,
                                                            channel_multiplier=1), [idb], [idb])
        P.op('pool', lambda e: e.memset(self.ones.t[:], 1.0), (), [self.ones])
        CP(P, 'dve', self.rwb, self.rwb.t[:], self.c('rw').rearrange("p (a b) -> p a b", a=8), [self.cf])
        ACT(P, self.cact, self.cact.t[:], self.c('cT').rearrange("p (a b) -> p a b", a=8), AF.Silu, [self.cf])
        ar = self.ar
        ar.reset()
        wb = [ar.carve("wada%d" % i, [8, 512], BF16) for i in range(2)]
        self.mod_layer(0, wb, 512)
        self.mod_deferred = (('ffn', 0) in self.phases) and (('mix', 1) in self.phases or ('ffn', 1) in self.phases) \
            and self.phases.index(('ffn', 0)) < min(self.phases.index(p) for p in self.phases if p[1] == 1)
        if not self.mod_deferred:
            self.mod_layer(1, wb, 512)
        P.barrier()

    def mod_layer(self, l, wb, W):
        for _ in self.mod_layer_gen(l, wb, W):
            pass

    def mod_layer_gen(self, l, wb, W):
        P = self.P
        pm = self.PB[0]
        nsub = W // 128
        nblk = 6 * D // W

        def load(blk):
            w = wb[blk % 2]
            P.dma_cast(w.t[:, :, 0:W], self.ada_w[l, :, blk * W:(blk + 1) * W].rearrange("(kc p) n -> p kc n", p=128),
                       writes=[w])

        load(0)
        for blk in range(nblk):
            if blk + 1 < nblk:
                load(blk + 1)
            yield
            w = wb[blk % 2]
            for oc in range(nsub):
                j = blk * nsub + oc
                o = pm.t[:, oc * NSEQ:(oc + 1) * NSEQ]
                for kc in range(8):
                    MM(P, pm, o, w.t[:, kc, oc * 128:(oc + 1) * 128], self.cact.t[:, kc, :], [w, self.cact],
                       kc == 0, kc == 7)
                ACT(P, self.modv, self.modv.t[:, l, j, :], o, AF.Identity, [pm, self.cf],
                    bias=self.c('ada_b', l * 48 + j, 1))
        for b in range(NSEQ):
            g = lambda w_: self.c('gains', (l * 4 + w_) * 8, 8)
            d = self.drv
            STT(P, 'dve', d, d.t[:, l, b, 0, :], self.modv.t[:, l, 8:16, b], 1.0, g(0), ALU.add, ALU.mult,
                [self.modv, self.cf])
            TT(P, 'dve', d, d.t[:, l, b, 1, :], self.modv.t[:, l, 16:24, b], g(1), ALU.mult, [self.modv, self.cf])
            STT(P, 'dve', d, d.t[:, l, b, 2, :], self.modv.t[:, l, 32:40, b], 1.0, g(2), ALU.add, ALU.mult,
                [self.modv, self.cf])
            TT(P, 'dve', d, d.t[:, l, b, 3, :], self.modv.t[:, l, 40:48, b], g(3), ALU.mult, [self.modv, self.cf])

    def rsq(self, buf, ap):
        P = self.P
        ACT(P, buf, ap, ap, AF.Sqrt, [buf])
        P.op('dve', lambda e: e.reciprocal(ap, ap), [buf], [buf])

    def rms_rstd(self, get_src, n, W, sqb, rstd):
        P = self.P
        pb = self.PB[0]
        for c in range(n):
            sb_, sap = get_src(c)
            q = sqb[c % 2]
            ACT(P, q, q.t[:, 0:W], sap, AF.Square, [sb_])
            MM(P, pb, pb.t[:, 0:W], self.ones.t[:], q.t[:, 0:W], [q, self.ones], c == 0, c == n - 1)
        TS(P, 'dve', rstd, rstd.t[:, 0:W], pb.t[:, 0:W], 1.0 / (128 * n), EPS, ALU.mult, ALU.add, [pb])
        self.rsq(rstd, rstd.t[:, 0:W])

    def prenorm(self, s, l, which, t0, hm, hoff, sqb, rstd, tmp):
        P = self.P
        xT = self.xTt[t0 // 512]
        self.rms_rstd(lambda c: (xT, xT.t[:, c, t0:t0 + 512]), 8, 512, sqb, rstd)
        Aidx = 0 if which == 0 else 2
        shbase = 0 if which == 0 else 24
        for c in range(8):
            tb = tmp[c % 2]
            STT(P, 'dve', tb, tb.t[:], xT.t[:, c, t0:t0 + 512], self.drv.t[:, l, s, Aidx, c:c + 1], rstd.t[:],
                ALU.mult, ALU.mult, [xT, self.drv, rstd])
            ACT(P, hm, hm.t[:, c, hoff:hoff + 512], tb.t[:], AF.Identity, [tb, self.modv],
                bias=self.modv.t[:, l, shbase + c, s:s + 1])

    def postnorm_residual(self, s, l, which, t0, get_y, W, sqb, rstd, tmp):
        P = self.P
        xT = self.xTt[t0 // 512]
        self.rms_rstd(get_y, 8, W, sqb, rstd)
        Gidx = 1 if which == 0 else 3
        for c in range(8):
            tb = tmp[c % 2]
            yb, yap = get_y(c)
            TT(P, 'dve', tb, tb.t[:, 0:W], yap, rstd.t[:, 0:W], ALU.mult, [yb, rstd])
            STT(P, 'dve', xT, xT.t[:, c, t0:t0 + W], tb.t[:, 0:W], self.drv.t[:, l, s, Gidx, c:c + 1],
                xT.t[:, c, t0:t0 + W], ALU.mult, ALU.add, [tb, self.drv, xT])

    def group_norm_tm(self, l, src, src_ap, chunk0, mixT, tokoff, tmps):
        P = self.P
        junk, ssq, on = tmps
        ACT(P, junk, junk.t[:, 256:512], src_ap, AF.Square, [src], acc=ssq.t[:, 0:1], wr=[junk, ssq])
        TS(P, 'dve', ssq, ssq.t[:, 1:2], ssq.t[:, 0:1], 1.0 / 256, EPS, ALU.mult, ALU.add, [ssq])
        self.rsq(ssq, ssq.t[:, 1:2])
        TS(P, 'dve', on, on.t[:, 0:256], src_ap, ssq.t[:, 1:2], None, ALU.mult, None, [src, ssq])
        pt = self.PB[7]
        ptv = pt.t[:].bitcast(BF16)
        for cc in range(2):
            TR(P, pt, ptv[:, cc * 128:(cc + 1) * 128], on.t[:, cc * 128:(cc + 1) * 128], self.ident.t[:],
               [on, self.ident])
            ACT(P, mixT, mixT.t[:, chunk0 + cc, tokoff:tokoff + 128], ptv[:, cc * 128:(cc + 1) * 128], AF.Identity,
                [pt, self.cf], scale=self.c('ggain', l * 8 + chunk0 + cc, 1))

    def mixer(self, s, l):
        P = self.P
        ar = self.ar
        ar.reset()
        cv = ar.carve
        hm = cv("hm", [8, 512], BF16)
        mixT = cv("mixT", [8, 512], BF16)
        wreg = cv("wreg", [8, 1024], BF16)
        wblk = [Buf("wblk0", wreg.t[:, :, 0:512]), Buf("wblk1", wreg.t[:, :, 512:1024])]
        qT = cv("qT", [4, 512], BF16)
        kT = cv("kT", [4, S], BF16)
        vaug = cv("vaug", [16, 8, 65], BF16)
        glu = cv("glu", [2, 542], F32)
        biasT = cv("biasT", [4, 512], BF16, parts=8)
        posi = cv("posi", [512], I32)
        cosT = cv("cosT", [512], F32)
        sinS = cv("sinS", [512], F32)
        sqb = [cv("sq%d" % i, [512], BF16) for i in range(2)]
        rstd = cv("rstd", [512], F32)
        tmp = [cv("tmp%d" % i, [512], F32) for i in range(4)]
        PT = [cv("PT%d" % i, [512], BF16) for i in range(5)]
        gl = cv("gl", [512], F32)
        vgn = cv("vgn", [256], BF16)
        wsTm = cv("wsTm", [4, 128], BF16)
        bb = cv("bb", [256], F32)
        stt = cv("stt", [8], F32)
        ssq = cv("ssq", [4], F32)
        on = cv("on", [256], BF16)
        ocb = cv("ocb", [256], F32)
        og = cv("og", [4, 256], F32)
        rec = cv("rec", [4], F32)
        kms = cv("kms", [2, 8], F32)
        kmb = cv("kmb", [2, 8], BF16)
        gm = cv("gm", [32], F32)
        top8 = cv("top8", [4, 8], F32)
        sel = cv("sel", [32], F32)
        bq = cv("bq", [32], BF16)
        acc = [cv("acc%d" % i, [512], F32) for i in range(2)]
        ybf = cv("ybf", [2, 512], BF16)
        PB = self.PB
        cf = self.cf
        gnt = (gl, ssq, on)

        P.dma_cast(wsTm.t[:], self.wsT[l].rearrange("g s t -> s g t"), writes=[wsTm])
        for g in range(4):
            TT(P, 'dve', wsTm, wsTm.t[:, g, :], wsTm.t[:, g, :], self.tri.t[:], ALU.mult, [wsTm, self.tri])
            TS(P, 'dve', bb, bb.t[:, g * 64:(g + 1) * 64], self.c('ones'), self.c('gb', l * 4 + g, 1), None, ALU.mult,
               None, [cf])
        P.op('dve', lambda e: e.memset(vaug.t[:, :, :, 64:65], 1.0), (), [vaug])
        P.op('dve', lambda e: e.memset(glu.t[:, :, 0:30], 0.0), (), [glu])
        P.op('dve', lambda e: e.memset(kms.t[:], 0.0), (), [kms])

        wsrc = self.w_in[l]
        state = {'issued': 0}

        def issue_to(n):
            while state['issued'] < n:
                i = state['issued']
                b_ = i % 7
                w = wblk[i % 2]
                P.dma_cast(w.t[:], wsrc[:, b_ * 512:(b_ + 1) * 512].rearrange("(kc p) n -> p kc n", p=128), writes=[w])
                state['issued'] += 1

        for tt in range(4):
            t0 = tt * 512
            posf, angt = tmp[2], tmp[3]
            fi_, gg_ = tmp[0], tmp[1]
            posi2 = Buf("posi2", posi.t)
            P.dma(posi.t[:], self.posrep[s, :, t0:t0 + 512], writes=[posi])
            CP(P, 'dve', posf, posf.t[:], posi.t[:], [posi])
            TS(P, 'dve', posf, posf.t[:], posf.t[:], self.c('invf'), None, ALU.mult, None, [posf, cf])
            CP(P, 'dve', posi, posi.t[:], posf.t[:], [posf])
            CP(P, 'dve', fi_, fi_.t[:], posi.t[:], [posi])
            TT(P, 'dve', angt, angt.t[:], posf.t[:], fi_.t[:], ALU.subtract, [posf, fi_])
            TS(P, 'dve', gg_, gg_.t[:], angt.t[:], 0.5, None, ALU.is_gt, None, [angt])
            TT(P, 'dve', angt, angt.t[:], angt.t[:], gg_.t[:], ALU.subtract, [angt, gg_])
            TS(P, 'dve', gg_, gg_.t[:], angt.t[:], -0.5, None, ALU.is_lt, None, [angt])
            TT(P, 'dve', angt, angt.t[:], angt.t[:], gg_.t[:], ALU.add, [angt, gg_])
            ACT(P, sinS, sinS.t[:], angt.t[:], AF.Sin, [angt, cf], scale=self.c('sgn'))
            TS(P, 'dve', angt, angt.t[:], angt.t[:], 0.25, None, ALU.add, None, [angt])
            TS(P, 'dve', gg_, gg_.t[:], angt.t[:], 0.5, None, ALU.is_gt, None, [angt])
            TT(P, 'dve', angt, angt.t[:], angt.t[:], gg_.t[:], ALU.subtract, [angt, gg_])
            ACT(P, cosT, cosT.t[:], angt.t[:], AF.Sin, [angt], scale=6.28318)
            self.prenorm(s, l, 0, t0, hm, 0, sqb, rstd, tmp)
            if self.dbg_on and tt == 0:
                self.tap("cos", cosT, cosT.t[:], [128, 512])
                self.tap("sin", sinS, sinS.t[:], [128, 512])
            for b in range(7):
                bi = tt * 7 + b
                issue_to(min(bi + 2, (tt + 1) * 7))
                w = wblk[bi % 2]
                if b < 5:
                    for cc in range(2):
                        pa = PB[1 + 2 * self.nxt('pp', 2)]
                        pbk = PB[2] if pa is PB[1] else PB[4]
                        for kc in range(8):
                            MM(P, pa, pa.t[:], w.t[:, kc, cc * 128:(cc + 1) * 128], hm.t[:, kc, :], [w, hm], kc == 0, kc == 7)
                        for kc in range(8):
                            MM(P, pbk, pbk.t[:], w.t[:, kc, 256 + cc * 128:256 + (cc + 1) * 128], hm.t[:, kc, :], [w, hm],
                               kc == 0, kc == 7)
                        if b < 4:
                            t1, t2 = tmp[0], tmp[1]
                            TT(P, 'dve', t1, t1.t[:], pa.t[:], cosT.t[:], ALU.mult, [pa, cosT])
                            TT(P, 'dve', t2, t2.t[:], pbk.t[:], sinS.t[:], ALU.mult, [pbk, sinS])
                            if b % 2 == 0:
                                dst, dap = qT, qT.t[:, (b // 2) * 2 + cc, :]
                            else:
                                dst, dap = kT, kT.t[:, (b // 2) * 2 + cc, t0:t0 + 512]
                            TT(P, 'pool', dst, dap, t1.t[:], t2.t[:], ALU.add, [t1, t2])
                        else:
                            t1 = tmp[2]
                            ACT(P, t1, t1.t[:], pbk.t[:], AF.Sigmoid, [pbk])
                            TT(P, 'dve', glu, glu.t[:, cc, 30:542], pa.t[:], t1.t[:], ALU.mult, [pa, t1])
                else:
                    for sub in range(4):
                        pc = PB[5 + self.nxt('pc', 2)]
                        for kc in range(8):
                            MM(P, pc, pc.t[:], hm.t[:, kc, sub * 128:(sub + 1) * 128], w.t[:, kc, :], [w, hm], kc == 0, kc == 7)
                        if b == 5:
                            ACT(P, vaug, vaug.t[:, tt * 4 + sub, :, 0:64], pc.t[:].rearrange("p (h d) -> p h d", h=8),
                                AF.Copy, [pc])
                        else:
                            self.cpath(l, pc, sub, mixT, gl, vgn, wsTm, bb, stt, gnt, ocb, tmp)
            P.dma_cast(wreg.t[:], self.w_out[l].rearrange("(kc p) n -> p kc n", p=128), writes=[wblk[0], wblk[1]])
            if self.dbg_on and tt == 0:
                self.tap("q", qT, qT.t[:], [128, 4, 512], BF16)
                self.tap("k", kT, kT.t[:, :, 0:512], [128, 4, 512], BF16)
                self.tap("glu", glu, glu.t[:], [128, 2, 542])
                self.tap("v", vaug, vaug.t[:, 0:4, :, :], [128, 4, 8, 65], BF16)
            self.gating(tt, qT, kT, kms, kmb, gm, top8, sel, bq, biasT)
            dp = self.dpath(l, glu, acc, ybf, rstd, tmp, mixT)
            if self.dbg_on and tt == 1:
                self.tap("biasT", biasT, biasT.t[:], [8, 4, 512], BF16)
            for G in range(2):
                self.attn_group(G, tt, qT, kT, vaug, biasT, PT, og, rec, dp)
                if self.dbg_on and tt == 1:
                    self.tap("og%d" % G, og, og.t[:], [128, 4, 256])
                for qs in range(4):
                    self.group_norm_tm(l, og, og.t[:, qs, :], G * 2, mixT, qs * 128, gnt)
            for _ in dp:
                pass
            if self.dbg_on and tt == 1:
                self.tap("mixT", mixT, mixT.t[:], [128, 8, 512], BF16)
            for hf in range(2):
                c0 = hf * 256

                def ysrc(oc):
                    return PB[1 + oc // 2], PB[1 + oc // 2].t[:, (oc % 2) * 256:(oc % 2) * 256 + 256]
                for oc in range(8):
                    yb, yap = ysrc(oc)
                    for kc in range(8):
                        MM(P, yb, yap, wreg.t[:, kc, oc * 128:(oc + 1) * 128], mixT.t[:, kc, c0:c0 + 256],
                           [wblk[0], wblk[1], mixT], kc == 0, kc == 7)
                self.postnorm_residual(s, l, 0, t0 + c0, ysrc, 256, sqb, rstd, tmp)
        P.barrier()

    def cpath(self, l, pc, sub, mixT, gl, vgn, wsTm, bb, stt, gn_tmps, ocb, tmp):
        P = self.P
        tA, tB = tmp[2], tmp[3]
        ACT(P, tA, tA.t[:], pc.t[:], AF.Square, [pc])
        TS(P, 'pool', tA, tA.t[:], tA.t[:], 0.044715, 1.0, ALU.mult, ALU.add, [tA])
        TT(P, 'dve', tA, tA.t[:], tA.t[:], pc.t[:], ALU.mult, [tA, pc])
        ACT(P, tB, tB.t[:], tA.t[:], AF.Sigmoid, [tA], scale=1.5957691216057308)
        TT(P, 'dve', gl, gl.t[:], pc.t[:], tB.t[:], ALU.mult, [pc, tB])
        P.op('dve', lambda e: e.bn_stats(stt.t[:, 0:6], gl.t[:, 256:512]), [gl], [stt])
        P.op('dve', lambda e: e.bn_aggr(stt.t[:, 6:8], stt.t[:, 0:6]), [stt], [stt])
        TS(P, 'dve', stt, stt.t[:, 7:8], stt.t[:, 7:8], EPS, None, ALU.add, None, [stt])
        self.rsq(stt, stt.t[:, 7:8])
        TS(P, 'dve', vgn, vgn.t[:], gl.t[:, 256:512], stt.t[:, 6:7], stt.t[:, 7:8], ALU.subtract, ALU.mult, [gl, stt])
        pz = self.PB[7]
        for g in range(4):
            MM(P, pz, pz.t[:, 256 + g * 64:256 + (g + 1) * 64], wsTm.t[:, g, :], vgn.t[:, g * 64:(g + 1) * 64], [wsTm, vgn])
        TT(P, 'dve', ocb, ocb.t[:], pz.t[:, 256:512], bb.t[:], ALU.add, [pz, bb])
        TT(P, 'dve', ocb, ocb.t[:], ocb.t[:], gl.t[:, 0:256], ALU.mult, [ocb, gl])
        self.group_norm_tm(l, ocb, ocb.t[:], 4, mixT, sub * 128, gn_tmps)

    def dpath(self, l, glu, acc, ybf, rstd, tmp, mixT):
        P = self.P
        cf = self.cf
        PB = self.PB
        for cc in range(2):
            eng = 'dve'
            a = acc[cc]
            wbase = (l * 2 + cc) * 31
            TS(P, eng, a, a.t[:], glu.t[:, cc, 0:512], self.c('convw', wbase, 1), self.c('convv', (l * 3 + 0) * 2 + cc, 1),
               ALU.mult, ALU.add, [glu, cf])
            for j in range(1, 31):
                STT(P, eng, a, a.t[:], glu.t[:, cc, j:j + 512], self.c('convw', wbase + j, 1), a.t[:], ALU.mult, ALU.add,
                    [glu, cf, a])
                if j % 8 == 0:
                    yield
        for cc in range(2):
            CP(P, 'dve', glu, glu.t[:, cc, 0:30], glu.t[:, cc, 512:542], [glu])
        p1, p2 = PB[5], PB[6]
        for cc in range(2):
            ACT(P, ybf, ybf.t[:, cc, :], acc[cc].t[:], AF.Copy, [acc[cc]])
            MM(P, p1, p1.t[:], self.ones.t[:], ybf.t[:, cc, :], [ybf, self.ones], cc == 0, cc == 1)
        for cc in range(2):
            ACT(P, ybf, ybf.t[:, cc, :], acc[cc].t[:], AF.Square, [acc[cc]])
            MM(P, p2, p2.t[:], self.ones.t[:], ybf.t[:, cc, :], [ybf, self.ones], cc == 0, cc == 1)
        t1, mean = tmp[0], tmp[1]
        TS(P, 'dve', mean, mean.t[:], p1.t[:], 1.0 / 256, None, ALU.mult, None, [p1])
        TT(P, 'dve', t1, t1.t[:], mean.t[:], mean.t[:], ALU.mult, [mean])
        STT(P, 'dve', t1, t1.t[:], p2.t[:], 1.0 / 256, t1.t[:], ALU.mult, ALU.subtract, [p2, t1])
        TS(P, 'dve', rstd, rstd.t[:], t1.t[:], EPS, None, ALU.add, None, [t1])
        self.rsq(rstd, rstd.t[:])
        for cc in range(2):
            a = acc[cc]
            TT(P, 'dve', a, a.t[:], a.t[:], mean.t[:], ALU.subtract, [a, mean])
            TT(P, 'dve', a, a.t[:], a.t[:], rstd.t[:], ALU.mult, [a, rstd])
            ACT(P, a, a.t[:], a.t[:], AF.Silu, [a, cf], bias=self.c('convv', (l * 3 + 2) * 2 + cc, 1),
                scale=self.c('convv', (l * 3 + 1) * 2 + cc, 1))
        for cc in range(2):
            ACT(P, ybf, ybf.t[:, cc, :], acc[cc].t[:], AF.Square, [acc[cc]])
            MM(P, p1, p1.t[:], self.ones.t[:], ybf.t[:, cc, :], [ybf, self.ones], cc == 0, cc == 1)
        TS(P, 'dve', rstd, rstd.t[:], p1.t[:], 1.0 / 256, EPS, ALU.mult, ALU.add, [p1])
        self.rsq(rstd, rstd.t[:])
        for cc in range(2):
            a = acc[cc]
            TT(P, 'dve', a, a.t[:], a.t[:], rstd.t[:], ALU.mult, [a, rstd])
            ACT(P, mixT, mixT.t[:, 6 + cc, :], a.t[:], AF.Identity, [a, cf], scale=self.c('ggain', l * 8 + 6 + cc, 1))

    def gating(self, tt, qT, kT, kms, kmb, gm, top8, sel, bq, biasT):
        P = self.P
        cf = self.cf
        for cc in range(2):
            for j in (2 * tt, 2 * tt + 1):
                P.op('dve', lambda e, cc=cc, j=j: e.tensor_reduce(kms.t[:, cc, j:j + 1], kT.t[:, cc, j * 256:(j + 1) * 256],
                                                                  AX.X, ALU.add), [kT], [kms])
        TS(P, 'dve', kmb, kmb.t[:], kms.t[:], 1.0 / 256, None, ALU.mult, None, [kms])
        pg = self.PB[7]
        ptv = pg.t[:].bitcast(BF16)
        for sub in range(4):
            nq = (tt * 4 + sub) // 2
            for h in range(4):
                pb0 = 64 * (h % 2)
                MM(P, pg, pg.t[:, h * 8:(h + 1) * 8], qT.t[pb0:pb0 + 64, h // 2, sub * 128:(sub + 1) * 128],
                   kmb.t[pb0:pb0 + 64, h // 2, :], [qT, kmb])
            TT(P, 'dve', gm, gm.t[:], pg.t[:, 0:32], self.c('pastneg', nq * 32, 32), ALU.add, [pg, cf])
            for h in range(4):
                P.op('dve', lambda e, h=h: e.max(top8.t[:, h, :], gm.t[:, h * 8:(h + 1) * 8]), [gm], [top8])
            for h in range(4):
                TS(P, 'dve', sel, sel.t[:, h * 8:(h + 1) * 8], gm.t[:, h * 8:(h + 1) * 8], top8.t[:, h, 2:3], None, ALU.is_ge,
                   None, [gm, top8])
            TT(P, 'dve', sel, sel.t[:], sel.t[:], self.c('past', nq * 32, 32), ALU.mult, [sel, cf])
            TT(P, 'dve', sel, sel.t[:], sel.t[:], self.c('own', nq * 32, 32), ALU.add, [sel, cf])
            TS(P, 'dve', bq, bq.t[:], sel.t[:], BIG, -BIG, ALU.mult, ALU.add, [sel])
            for h in range(4):
                TR(P, pg, ptv[0:8, 128 + h * 128:128 + (h + 1) * 128], bq.t[:, h * 8:(h + 1) * 8], self.ident.t[:],
                   [bq, self.ident])
            CP(P, 'dve', biasT, biasT.t[:, :, sub * 128:(sub + 1) * 128],
               ptv[0:8, 128:640].rearrange("p (h q) -> p h q", h=4), [pg])

    def attn_group(self, G, qt, qT, kT, vaug, biasT, PT, ogb, rec, dp):
        P = self.P
        PB = self.PB
        nk = 4 * qt + 4
        steps = [(h, kt) for h in range(4) for kt in range(nk)]
        n = len(steps)
        LA = 3
        Sbanks = [PB[1], PB[2], PB[0], PB[7]]
        Es = {}

        def emit_S(i):
            h, kt = steps[i]
            cq = G * 2 + h // 2
            pb0 = 64 * (h % 2)
            Sb = Sbanks[i % 4]
            MM(P, Sb, Sb.t[:], kT.t[pb0:pb0 + 64, cq, kt * 128:(kt + 1) * 128], qT.t[pb0:pb0 + 64, cq, :], [kT, qT],
               True, G == 1)
            if G == 0:
                jb = kt // 2
                MM(P, Sb, Sb.t[:], self.indc.t[0:8, jb * 128:(jb + 1) * 128], biasT.t[0:8, h, :], [self.indc, biasT],
                   False, True)
            E = PT[i % 5]
            ACT(P, E, E.t[:], Sb.t[:], AF.Exp, [Sb], scale=0.125)
            dl = 512 * qt - 128 * kt
            if G == 1:
                mi = (dl + 384) // 128 if dl <= 512 else 8
                TT(P, 'dve', E, E.t[:], E.t[:], self.mB.t[:, mi, :], ALU.mult, [E, self.mB])
            elif dl <= 0:
                TT(P, 'dve', E, E.t[:], E.t[:], self.mA.t[:, (-dl) // 128, :], ALU.mult, [E, self.mA])
            Es[i] = E

        def emit_PV(i):
            h, kt = steps[i]
            O = PB[3 + h % 2]
            E = Es.pop(i)
            for qs in range(4):
                last = 4 * qt + qs
                if kt <= last:
                    MM(P, O, O.t[:, qs * 65:(qs + 1) * 65], E.t[:, qs * 128:(qs + 1) * 128], vaug.t[:, kt, G * 4 + h, :],
                       [E, vaug], kt == 0 and qs == 0, kt == last)
            if kt == nk - 1:
                O3 = O.t[:, 0:260].rearrange("p (q d) -> p q d", q=4)
                P.op('dve', lambda e: e.reciprocal(rec.t[:].rearrange("p (q o) -> p q o", o=1), O3[:, :, 64:65]), [O], [rec])
                for qs in range(4):
                    TS(P, 'dve', ogb, ogb.t[:, qs, h * 64:(h + 1) * 64], O.t[:, qs * 65:qs * 65 + 64], rec.t[:, qs:qs + 1],
                       None, ALU.mult, None, [O, rec])
                next(dp, None)

        for i in range(min(LA, n)):
            emit_S(i)
        for i in range(n):
            if i + LA < n:
                emit_S(i + LA)
            emit_PV(i)

    def ffn(self, s, l, last=False):
        P = self.P
        ar = self.ar
        PB = self.PB
        moe = (l == 1)
        nfb = (DFE if moe else DFF) // 256
        for grp in range(2):
            g0 = grp * 1024
            ar.reset()
            cv = ar.carve
            hT = cv("hT", [8, 1024], BF16)
            yacc = cv("yacc", [8, 1024], F32)
            act = [cv("act%d" % i, [2, 1024], BF16) for i in range(2)]
            wg = [cv("wg%d" % i, [8, 256], BF16) for i in range(2)]
            wu = [cv("wu%d" % i, [8, 256], BF16) for i in range(2)]
            wd = [cv("wd%d" % i, [2, 1024], BF16) for i in range(2)]
            sqb = [cv("sq%d" % i, [512], BF16) for i in range(2)]
            rstd = cv("rstd", [512], F32)
            tmp = [cv("tmp%d" % i, [512], F32) for i in range(4)]
            for tl in range(2):
                self.prenorm(s, l, 1, g0 + tl * 512, hT, tl * 512, sqb, rstd, tmp)
            if (not moe) and s == 0 and grp == 0 and self.mod_deferred:
                wbd = [cv("wadad%d" % i, [8, 512], BF16) for i in range(2)]
                modgen = self.mod_layer_gen(1, wbd, 512)
            else:
                modgen = iter(())
            if moe:
                gatesT = cv("gatesT", [1024], F32, parts=8)
                indf = cv("indf", [1024], F32, parts=8)
                gbc = cv("gbc", [2, 512], F32)
                lg = cv("lg", [8], F32)
                top8 = cv("top8", [8], F32)
                sm = cv("sm", [8], F32)
                e1 = cv("e1", [8], F32)
                e2 = cv("e2", [8], F32)
                P.dma(indf.t[:], self.indc_d, writes=[indf])
                self.router(hT, gatesT, lg, top8, sm, e1, e2)
                blocks = [(e, fb) for e in range(NE) for fb in range(nfb)]
            else:
                blocks = [(0, fb) for fb in range(nfb)]
            nb = len(blocks)

            def wsrcs(i):
                e_, fb_ = blocks[i]
                if moe:
                    return self.moe_wg[e_], self.moe_wu[e_], self.moe_wd[e_], fb_ * 256
                return self.ffn_wg, self.ffn_wu, self.ffn_wd, fb_ * 256

            def issue_gu(i):
                if i >= nb:
                    return
                sg_, su_, sd_, c0 = wsrcs(i)
                P.dma_cast(wg[i % 2].t[:], sg_[:, c0:c0 + 256].rearrange("(kc p) n -> p kc n", p=128), writes=[wg[i % 2]])
                P.dma_cast(wu[i % 2].t[:], su_[:, c0:c0 + 256].rearrange("(kc p) n -> p kc n", p=128), writes=[wu[i % 2]])

            def issue_d(i):
                if i >= nb:
                    return
                sg_, su_, sd_, c0 = wsrcs(i)
                P.dma_cast(wd[i % 2].t[:], sd_[c0:c0 + 256, :].rearrange("(fc p) n -> p fc n", p=128), writes=[wd[i % 2]])

            def gu_units(bi):
                e, fb = blocks[bi]
                wgb, wub, ab = wg[bi % 2], wu[bi % 2], act[bi % 2]
                if moe and fb == 0:
                    for tl in range(2):
                        pgb = PB[0]
                        MM(P, pgb, pgb.t[:], indf.t[0:8, e * 128:(e + 1) * 128], gatesT.t[0:8, tl * 512:(tl + 1) * 512],
                           [indf, gatesT])
                        ACT(P, gbc, gbc.t[:, tl, :], pgb.t[:], AF.Copy, [pgb])
                for fc in range(2):
                    for tl in range(2):
                        pg = PB[1 + 2 * self.nxt('fg', 2)]
                        pu = PB[2] if pg is PB[1] else PB[4]
                        for kc in range(8):
                            MM(P, pg, pg.t[:], wgb.t[:, kc, fc * 128:(fc + 1) * 128], hT.t[:, kc, tl * 512:(tl + 1) * 512],
                               [wgb, hT], kc == 0, kc == 7)
                        for kc in range(8):
                            MM(P, pu, pu.t[:], wub.t[:, kc, fc * 128:(fc + 1) * 128], hT.t[:, kc, tl * 512:(tl + 1) * 512],
                               [wub, hT], kc == 0, kc == 7)
                        t1 = tmp[self.nxt('ft', 2)]
                        ACT(P, t1, t1.t[:], pg.t[:], AF.Silu, [pg])
                        if moe:
                            TT(P, 'dve', t1, t1.t[:], t1.t[:], gbc.t[:, tl, :], ALU.mult, [t1, gbc])
                        TT(P, 'dve', ab, ab.t[:, fc, tl * 512:(tl + 1) * 512], t1.t[:], pu.t[:], ALU.mult, [t1, pu])
                        yield

            def down_parts(bi):
                wdb, ab = wd[bi % 2], act[bi % 2]
                for oc in range(8):
                    for tl in range(2):
                        py = PB[5 + self.nxt('fy', 3)]
                        for fc in range(2):
                            MM(P, py, py.t[:], wdb.t[:, fc, oc * 128:(oc + 1) * 128], ab.t[:, fc, tl * 512:(tl + 1) * 512],
                               [wdb, ab], fc == 0, fc == 1)
                        ya = yacc.t[:, oc, tl * 512:(tl + 1) * 512]
                        if bi == 0:
                            ACT(P, yacc, ya, py.t[:], AF.Copy, [py])
                        else:
                            TT(P, 'dve', yacc, ya, ya, py.t[:], ALU.add, [yacc, py])
                    if oc % 2 == 1:
                        yield

            issue_gu(0)
            issue_d(0)
            issue_gu(1)
            issue_d(1)
            for _ in gu_units(0):
                pass
            for bi in range(nb):
                g_it = gu_units(bi + 1) if bi + 1 < nb else iter(())
                d_it = down_parts(bi)
                issue_gu(bi + 2)
                for u in range(4):
                    next(g_it, None)
                    next(d_it, None)
                for _ in g_it:
                    pass
                for _ in d_it:
                    pass
                issue_d(bi + 2)
                next(modgen, None)
            for _ in modgen:
                pass
            for tl in range(2):
                self.postnorm_residual(s, l, 1, g0 + tl * 512, lambda c, tl=tl: (yacc, yacc.t[:, c, tl * 512:(tl + 1) * 512]),
                                       512, sqb, rstd, tmp)
            P.barrier()
            if last:
                for tile in (2 * grp, 2 * grp + 1):
                    self.xstore(s, tile)
                    if s + 1 < NSEQ:
                        self.xload(s + 1, tile)

    def router(self, hT, gatesT, lg, top8, sm, e1, e2):
        P = self.P
        pl = self.PB[7]
        for sub in range(8):
            for kc in range(8):
                MM(P, pl, pl.t[:, 0:8], hT.t[:, kc, sub * 128:(sub + 1) * 128], self.rwb.t[:, kc, :], [hT, self.rwb],
                   kc == 0, kc == 7)
            TT(P, 'dve', lg, lg.t[:], pl.t[:, 0:8], self.rb.t[:], ALU.add, [pl, self.rb])
            P.op('dve', lambda e: e.max(top8.t[:], lg.t[:]), [lg], [top8])
            TS(P, 'dve', sm, sm.t[:, 0:1], top8.t[:, 1:2], top8.t[:, 0:1], None, ALU.subtract, None, [top8])
            ACT(P, sm, sm.t[:, 1:2], sm.t[:, 0:1], AF.Exp, [sm])
            TS(P, 'dve', sm, sm.t[:, 2:3], sm.t[:, 1:2], 1.0, None, ALU.add, None, [sm])
            P.op('dve', lambda e: e.reciprocal(sm.t[:, 3:4], sm.t[:, 2:3]), [sm], [sm])
            TT(P, 'dve', sm, sm.t[:, 4:5], sm.t[:, 1:2], sm.t[:, 3:4], ALU.mult, [sm])
            TS(P, 'dve', e1, e1.t[:], lg.t[:], top8.t[:, 0:1], sm.t[:, 3:4], ALU.is_equal, ALU.mult, [lg, top8, sm])
            TS(P, 'dve', e2, e2.t[:], lg.t[:], top8.t[:, 1:2], sm.t[:, 4:5], ALU.is_equal, ALU.mult, [lg, top8, sm])
            TT(P, 'dve', e1, e1.t[:], e1.t[:], e2.t[:], ALU.add, [e1, e2])
            TR(P, pl, pl.t[0:8, 128:256], e1.t[:], self.identf.t[:], [e1, self.identf])
            CP(P, 'dve', gatesT, gatesT.t[:, sub * 128:(sub + 1) * 128], pl.t[0:8, 128:256], [pl])

    def xload(self, s, tile):
        a, b = tile * 512, (tile + 1) * 512
        self.P.dma(self.xT.t[:, :, a:b], self.xin[s].rearrange("(c p) t -> p c t", p=128)[:, :, a:b], writes=[self.xTt[tile]])

    def xstore(self, s, tile):
        a, b = tile * 512, (tile + 1) * 512
        self.P.dma(self.xout[s].rearrange("(c p) t -> p c t", p=128)[:, :, a:b], self.xT.t[:, :, a:b], reads=[self.xTt[tile]])

    def build(self):
        P = self.P
        self.setup()
        self.early_io = (self.phases[-1][0] == 'ffn')
        for tile in range(4):
            self.xload(0, tile)
        for s in range(NSEQ):
            if s > 0 and not self.early_io:
                for tile in range(4):
                    self.xload(s, tile)
            for pi, ph in enumerate(self.phases):
                if ph[0] == 'mix':
                    self.mixer(s, ph[1])
                else:
                    self.ffn(s, ph[1], last=(self.early_io and pi == len(self.phases) - 1))
            if not self.early_io:
                for tile in range(4):
                    self.xstore(s, tile)
                P.barrier()
        P.finish()
        return self.nc


def _consts():
    p = np.arange(128)[:, None]
    j = np.arange(512)[None, :]

    def wB(d):
        d = d.astype(np.int64)
        return (((d >= 0) & (d <= 128)).astype(np.float32) + ((d >= 0) & (d % 4 == 0) & (d <= 512)).astype(np.float32)
                + ((d >= 0) & (d % 16 == 0) & (d <= 2048)).astype(np.float32))

    mB = np.zeros((128, 9, 512), np.float32)
    for i in range(8):
        dl = -384 + 128 * i
        mB[:, i, :] = wB(dl + j - p)
    mB[:, 8, :] = ((j - p) % 16 == 0).astype(np.float32)
    mA = np.zeros((128, 4, 512), np.float32)
    for i in range(4):
        mA[:, i, :] = ((-128 * i + j - p) >= 0).astype(np.float32)
    tri = (np.arange(128)[:, None] <= np.arange(128)[None, :]).astype(np.float32)
    indc = np.zeros((8, 8, 128), np.float32)
    for jb in range(8):
        indc[jb, jb, :] = 1.0
    return mB.reshape(128, -1), mA.reshape(128, -1), tri, indc.reshape(8, 1024)


def _prep_shared(inp):
    f = lambda a: np.ascontiguousarray(np.asarray(a, dtype=np.float32))
    sh = {}
    mB, mA, tri, indc = _consts()
    sh.update(mB=mB, mA=mA, tri=tri, indc=indc)
    sh['ada_w'] = f(inp['ada_w'])
    w_in = np.asarray(inp['w_in'], np.float32)
    sl = lambda i: np.arange(i * 256, (i + 1) * 256)
    swp = np.concatenate([np.concatenate([np.arange(h * 64 + 32, h * 64 + 64), np.arange(h * 64, h * 64 + 32)]) for h in range(4)])
    qa, ka, va, qb, kb, vb, u, vg, ga, gg = [sl(i) for i in range(10)]
    cols = np.concatenate([qa, qa[swp], ka, ka[swp], qb, qb[swp], kb, kb[swp], ga, gg, va, vb, u, vg])
    sh['w_in'] = np.ascontiguousarray(w_in[:, :, cols])
    sh['w_out'] = f(inp['w_out'])
    sh['wsT'] = np.ascontiguousarray(np.asarray(inp['gmlp_ws'], np.float32).transpose(0, 1, 3, 2))
    sh['ffn_wg'] = f(inp['ffn_w_gate'][0])
    sh['ffn_wu'] = f(inp['ffn_w_up'][0])
    sh['ffn_wd'] = f(inp['ffn_w_down'][0])
    sh['moe_wg'] = f(inp['moe_w_gate'][0])
    sh['moe_wu'] = f(inp['moe_w_up'][0])
    sh['moe_wd'] = f(inp['moe_w_down'][0])
    sh['rbrep'] = np.ascontiguousarray(np.broadcast_to(np.asarray(inp['router_b'], np.float32)[0][None, :], (128, 8)))
    cf = np.zeros((128, NCF), np.float32)

    def put(name, arr):
        off, n = CM[name]
        cf[:, off:off + n] = np.asarray(arr, np.float32).reshape(128, n)

    half = 32
    invf = np.power(np.float32(10000.0), -np.arange(half, dtype=np.float32) / np.float32(half)).astype(np.float32)
    pidx = np.arange(128)
    put('invf', (invf[pidx % 32].astype(np.float64) / (2 * math.pi)).astype(np.float32))
    sgn = np.where((pidx % 64) < 32, -1.0, 1.0).astype(np.float32)
    put('sgn', (6.28318 * sgn).astype(np.float32))
    put('npsgn', (-math.pi * sgn).astype(np.float32))
    put('negpi', np.full(128, -math.pi, np.float32))
    gains = np.stack([np.asarray(inp[k], np.float32) for k in ('mix_pre_g', 'mix_post_g', 'ffn_pre_g', 'ffn_post_g')], 1)
    put('gains', gains.reshape(2, 4, 8, 128).transpose(3, 0, 1, 2))
    put('ggain', np.asarray(inp['group_out_g'], np.float32).reshape(2, 8, 128).transpose(2, 0, 1))
    put('gb', np.asarray(inp['gmlp_b'], np.float32).transpose(2, 0, 1))
    put('convw', np.asarray(inp['conv_w'], np.float32).reshape(2, 31, 2, 128).transpose(3, 0, 2, 1))
    cvv = np.stack([np.asarray(inp[k], np.float32) for k in ('conv_b', 'conv_ln_g', 'conv_ln_b')], 1)
    put('convv', cvv.reshape(2, 3, 2, 128).transpose(3, 0, 1, 2))
    put('ada_b', np.asarray(inp['ada_b'], np.float32).reshape(2, 48, 128).transpose(2, 0, 1))
    put('rw', np.asarray(inp['router_w'], np.float32)[0].reshape(8, 128, 8).transpose(1, 0, 2))
    n = np.arange(8)[:, None, None]
    jj = np.arange(8)[None, None, :]
    past = np.broadcast_to((jj < n), (8, 4, 8)).astype(np.float32)
    own = np.broadcast_to((jj == n), (8, 4, 8)).astype(np.float32)
    put('past', np.broadcast_to(past.reshape(1, -1), (128, 256)))
    put('own', np.broadcast_to(own.reshape(1, -1), (128, 256)))
    put('pastneg', np.broadcast_to(((1.0 - past) * -1e30).reshape(1, -1), (128, 256)))
    put('ones', np.ones((128, 64), np.float32))
    sh['_cf'] = cf
    return sh


def _core_inputs(inp, sh, core):
    b0 = core * NSEQ
    m = {k: v for k, v in sh.items() if not k.startswith('_')}
    x = np.asarray(inp['x'], np.float32)[b0:b0 + NSEQ]
    m['xT'] = np.ascontiguousarray(x.transpose(0, 2, 1))
    pos = np.asarray(inp['positions'], np.int32)[b0:b0 + NSEQ]
    m['posrep'] = np.ascontiguousarray(np.broadcast_to(pos[:, None, :], (NSEQ, 128, S)))
    cf = sh['_cf'].copy()
    off, n = CM['cT']
    c = np.asarray(inp['c'], np.float32)[b0:b0 + NSEQ]
    cf[:, off:off + n] = c.reshape(NSEQ, 8, 128).transpose(2, 1, 0).reshape(128, n)
    m['cf'] = cf
    return m


ALL_PHASES = [('mix', 0), ('ffn', 0), ('mix', 1), ('ffn', 1)]


def kernel(**inputs):
    sh = _prep_shared(inputs)
    nc = K(ALL_PHASES).build()
    in_maps = [_core_inputs(inputs, sh, c) for c in range(NCORES)]
    res = run_bass_kernel_spmd(nc, in_maps, core_ids=list(range(NCORES)))
    out = np.concatenate([np.asarray(r["outT"], np.float32).transpose(0, 2, 1) for r in res.results], axis=0)
    return np.ascontiguousarray(out.astype(np.float32))
```

```python
from contextlib import ExitStack
import math
import numpy as np
import concourse.bass as bass
import concourse.mybir as mybir
from concourse.bass_utils import run_bass_kernel_spmd

F32 = mybir.dt.float32
BF16 = mybir.dt.bfloat16
I32 = mybir.dt.int32
AF = mybir.ActivationFunctionType
ALU = mybir.AluOpType
AX = mybir.AxisListType

S = 2048
D = 1024
NCORES = 8
NSEQ = 2
DFF = 2816
DFE = 3584
NE = 8
EPS = 1e-6
BIG = 30000.0
TWO_PI = 2.0 * math.pi

ENGS = ('pe', 'act', 'dve', 'pool', 'sp')
N_DMA_SEMS = 40
N_SP_SEMS = 8


class Buf:
    def __init__(self, name, t=None):
        self.name = name
        self.t = t
        self.last_w = None
        self.readers = {}


class _Op:
    __slots__ = ('eng', 'fn', 'deps', 'idx', 'needed', 'dma', 'sem', 'val', 'cnt')

    def __init__(self, eng, fn, dma):
        self.eng = eng
        self.fn = fn
        self.deps = []
        self.needed = False
        self.dma = dma
        self.sem = None
        self.val = 0
        self.cnt = 0


class Prog:
    def __init__(self, nc):
        self.nc = nc
        self.stack = ExitStack()
        self.ops = {e: [] for e in ENGS}
        self.n_dma = 0
        self.n_dma_sp = 0
        self.dma_last = [None] * N_DMA_SEMS
        self.dma_uses = [0] * N_DMA_SEMS
        self.pending = {e: [] for e in ENGS}

    def sb(self, name, shape, dt):
        t = self.stack.enter_context(self.nc.sbuf_tensor(name, list(shape), dt))
        return Buf(name, t)

    def ps(self, name, shape, dt):
        t = self.stack.enter_context(self.nc.psum_tensor(name, list(shape), dt))
        return Buf(name, t)

    def _add(self, eng, fn, reads, writes, dma=False):
        op = _Op(eng, fn, dma)
        op.idx = len(self.ops[eng])
        deps = list(self.pending[eng])
        self.pending[eng] = []
        for b in reads:
            if b.last_w is not None:
                deps.append(b.last_w)
        for b in writes:
            if b.last_w is not None:
                deps.append(b.last_w)
            deps.extend(b.readers.values())
        for b in reads:
            b.readers[('dma', id(op)) if dma else eng] = op
        for b in writes:
            b.last_w = op
            b.readers = {}
        seen = set()
        for d in deps:
            if d is op or id(d) in seen:
                continue
            seen.add(id(d))
            if (not d.dma) and (not dma) and d.eng == eng == 'pe':
                continue
            op.deps.append(d)
            d.needed = True
        self.ops[eng].append(op)
        return op

    def op(self, eng, fn, reads=(), writes=()):
        return self._add(eng, fn, reads, writes)

    def _dma(self, eng, fn, reads, writes):
        op = self._add(eng, fn, reads, writes, dma=True)
        if eng == 'sp':
            k = self.n_dma_sp % N_SP_SEMS
            self.n_dma_sp += 1
        else:
            k = N_SP_SEMS + self.n_dma % (N_DMA_SEMS - N_SP_SEMS)
            self.n_dma += 1
        prev = self.dma_last[k]
        if prev is not None:
            op.deps.append(prev)
        self.dma_uses[k] += 1
        op.sem = k
        op.val = 16 * self.dma_uses[k]
        self.dma_last[k] = op
        return op

    def dma(self, out, in_, reads=(), writes=()):
        return self._dma('sp', lambda e: e.dma_start(out=out, in_=in_), reads, writes)

    def dma_cast(self, out, in_, reads=(), writes=()):
        return self._dma('pool', lambda e: e.dma_start(out=out, in_=in_), reads, writes)

    def barrier(self):
        lasts = [o for o in self.dma_last if o is not None]
        for e in ENGS:
            for o in reversed(self.ops[e]):
                if not o.dma:
                    o.needed = True
                    lasts.append(o)
                    break
        for e in ENGS:
            self.pending[e] = list(lasts)

    def finish(self):
        nc = self.nc
        self.barrier()
        fin = _Op('sp', None, False)
        fin.deps = list(self.pending['sp'])
        self.ops['sp'].append(fin)
        for e in ENGS:
            c = 0
            for o in self.ops[e]:
                if (not o.dma) and o.needed:
                    c += 1
                    o.cnt = c
        st = self.stack
        esem = {e: st.enter_context(nc.semaphore("s_" + e)) for e in ENGS}
        dsem = [st.enter_context(nc.semaphore("d%d" % k)) for k in range(N_DMA_SEMS)]
        block = st.enter_context(nc.Block())
        ops = self.ops

        def run(ename, eng):
            waited = {}
            for o in ops[ename]:
                need = {}
                for d in o.deps:
                    if d.dma:
                        key = ('d', d.sem)
                        v = d.val
                    else:
                        key = ('e', d.eng)
                        v = d.cnt
                    if v > need.get(key, 0):
                        need[key] = v
                for key, v in need.items():
                    if waited.get(key, 0) >= v:
                        continue
                    waited[key] = v
                    eng.wait_ge(dsem[key[1]] if key[0] == 'd' else esem[key[1]], v)
                if o.fn is None:
                    continue
                ins = o.fn(eng)
                if o.dma:
                    ins.then_inc(dsem[o.sem], 16)
                elif o.needed:
                    ins.then_inc(esem[ename], 1)

        @block.tensor
        def _(eng):
            run('pe', eng)

        @block.scalar
        def _(eng):
            run('act', eng)

        @block.vector
        def _(eng):
            run('dve', eng)

        @block.gpsimd
        def _(eng):
            run('pool', eng)

        @block.sync
        def _(eng):
            run('sp', eng)

        st.close()


_DTSIZE = {F32: 4, BF16: 2, I32: 4}


class Arena:
    def __init__(self, P, name, nwords):
        self.t = P.sb(name, [128, nwords], F32).t
        self.cap = nwords
        self.off = 0

    def reset(self):
        self.off = 0

    def carve(self, name, shape, dt, parts=128):
        n = 1
        for s_ in shape:
            n *= s_
        words = (n * _DTSIZE[dt] + 3) // 4
        assert self.off + words <= self.cap, (name, self.off, words, self.cap)
        ap = self.t[0:parts, self.off:self.off + words]
        if dt != F32:
            ap = ap.bitcast(dt)
        if len(shape) == 2:
            ap = ap.rearrange("p (a b) -> p a b", a=shape[0])
        elif len(shape) == 3:
            ap = ap.rearrange("p (a b c) -> p a b c", a=shape[0], b=shape[1])
        self.off += words
        return Buf(name, ap)


def MM(P, ob, o, a, b, rd, st=True, sp=True):
    P.op('pe', lambda e: e.matmul(o, a, b, start=st, stop=sp), rd, [ob])


def TR(P, ob, o, a, ident, rd):
    P.op('pe', lambda e: e.transpose(o, a, ident), rd, [ob])


def ACT(P, ob, o, i, f, rd, bias=0.0, scale=1.0, acc=None, wr=None):
    if acc is None:
        P.op('act', lambda e: e.activation(o, i, f, bias=bias, scale=scale), rd, wr or [ob])
    else:
        P.op('act', lambda e: e.activation(o, i, f, bias=bias, scale=scale, accum_out=acc), rd, wr or [ob])


def TT(P, eng, ob, o, a, b, op, rd):
    P.op(eng, lambda e: e.tensor_tensor(o, a, b, op), rd, [ob])


def TS(P, eng, ob, o, a, s1, s2, op0, op1, rd):
    if op1 is None:
        P.op(eng, lambda e: e.tensor_scalar(o, a, s1, None, op0), rd, [ob])
    else:
        P.op(eng, lambda e: e.tensor_scalar(o, a, s1, s2, op0, op1), rd, [ob])


def STT(P, eng, ob, o, a, s, b, op0, op1, rd):
    P.op(eng, lambda e: e.scalar_tensor_tensor(o, a, s, b, op0, op1), rd, [ob])


def CP(P, eng, ob, o, i, rd):
    P.op(eng, lambda e: e.tensor_copy(o, i), rd, [ob])


def _cmap():
    m = {}
    off = 0
    for name, n in (('invf', 1), ('sgn', 1), ('npsgn', 1), ('negpi', 1), ('gains', 64), ('ggain', 16),
                    ('gb', 8), ('convw', 124), ('convv', 12), ('ada_b', 96), ('rw', 64),
                    ('pastneg', 256), ('past', 256), ('own', 256), ('cT', 8 * NSEQ), ('ones', 64)):
        m[name] = (off, n)
        off += n
    return m, off


CM, NCF = _cmap()


class K:
    def __init__(self, phases, dbg=False):
        self.phases = phases
        nc = bass.Bass("TRN2", target_bir_lowering=False)
        self.nc = nc
        P = Prog(nc)
        self.P = P

        def din(name, shape, dt=F32):
            return nc.dram_tensor(name, list(shape), dt, kind="ExternalInput").ap()

        self.xin = din("xT", [NSEQ, D, S])
        self.xout = nc.dram_tensor("outT", [NSEQ, D, S], F32, kind="ExternalOutput").ap()
        self.posrep = din("posrep", [NSEQ, 128, S], I32)
        self.cf_d = din("cf", [128, NCF])
        self.mB_d = din("mB", [128, 9 * 512])
        self.mA_d = din("mA", [128, 4 * 512])
        self.tri_d = din("tri", [128, 128])
        self.indc_d = din("indc", [8, 1024])
        self.rb_d = din("rbrep", [128, 8])
        self.ada_w = din("ada_w", [2, D, 6 * D])
        self.w_in = din("w_in", [2, D, 3584])
        self.w_out = din("w_out", [2, D, D])
        self.wsT = din("wsT", [2, 4, 128, 128])
        self.ffn_wg = din("ffn_wg", [D, DFF])
        self.ffn_wu = din("ffn_wu", [D, DFF])
        self.ffn_wd = din("ffn_wd", [DFF, D])
        self.moe_wg = din("moe_wg", [NE, D, DFE])
        self.moe_wu = din("moe_wu", [NE, D, DFE])
        self.moe_wd = din("moe_wd", [NE, DFE, D])
        self.dbg = {}
        self.dbg_on = dbg

        self.xT = P.sb("xTs", [128, 8, S], F32)
        self.xTt = [Buf("xTt%d" % i, self.xT.t) for i in range(4)]
        self.cf = P.sb("cfs", [128, NCF], F32)
        self.ident = P.sb("ident", [128, 128], BF16)
        self.identf = P.sb("identf", [128, 128], F32)
        self.ones = P.sb("ones", [128, 128], BF16)
        self.mB = P.sb("mBs", [128, 9, 512], BF16)
        self.mA = P.sb("mAs", [128, 4, 512], BF16)
        self.tri = P.sb("tris", [128, 128], BF16)
        self.indc = P.sb("indcs", [8, 1024], BF16)
        self.rb = P.sb("rbs", [128, 8], F32)
        self.cact = P.sb("cact", [128, 8, NSEQ], BF16)
        self.modv = P.sb("modv", [128, 2, 48, NSEQ], F32)
        self.drv = P.sb("drv", [128, 2, NSEQ, 4, 8], F32)
        self.rwb = P.sb("rwb", [128, 8, 8], BF16)
        self.PB = [P.ps("pb%d" % i, [128, 512], F32) for i in range(8)]
        self.ar = Arena(P, "arena", 30000)
        self.rot = {}

    def c(self, name, a=0, n=None):
        off, nn = CM[name]
        if n is None:
            n = nn - a
        return self.cf.t[:, off + a:off + a + n]

    def nxt(self, key, n):
        v = self.rot.get(key, 0)
        self.rot[key] = (v + 1) % n
        return v

    def tap(self, name, buf, ap, shape, dt=F32):
        if not self.dbg_on or name in self.dbg:
            return
        d = self.nc.dram_tensor("dbg_" + name, list(shape), dt, kind="ExternalOutput").ap()
        self.dbg[name] = d
        self.P.dma(d, ap, reads=[buf])

    def setup(self):
        P = self.P
        P.dma(self.cf.t[:], self.cf_d, writes=[self.cf])
        P.dma(self.rb.t[:], self.rb_d, writes=[self.rb])
        P.dma_cast(self.mB.t[:], self.mB_d.rearrange("p (a b) -> p a b", a=9), writes=[self.mB])
        P.dma_cast(self.mA.t[:], self.mA_d.rearrange("p (a b) -> p a b", a=4), writes=[self.mA])
        P.dma_cast(self.tri.t[:], self.tri_d, writes=[self.tri])
        P.dma_cast(self.indc.t[:], self.indc_d, writes=[self.indc])
        for idb, val in ((self.ident, 1.0), (self.identf, 1.0)):
            P.op('pool', lambda e, t=idb.t: e.memset(t[:], 1.0), (), [idb])
            P.op('pool', lambda e, t=idb.t: e.affine_select(t[:], t[:], [[-1, 128]], ALU.is_equal, 0.0, base=0,
                                                            channel_multiplier=1), [idb], [idb])
        P.op('pool', lambda e: e.memset(self.ones.t[:], 1.0), (), [self.ones])
        CP(P, 'dve', self.rwb, self.rwb.t[:], self.c('rw').rearrange("p (a b) -> p a b", a=8), [self.cf])
        ACT(P, self.cact, self.cact.t[:], self.c('cT').rearrange("p (a b) -> p a b", a=8), AF.Silu, [self.cf])
        ar = self.ar
        ar.reset()
        wb = [ar.carve("wada%d" % i, [8, 512], BF16) for i in range(2)]
        self.mod_layer(0, wb, 512)
        self.mod_deferred = (('ffn', 0) in self.phases) and (('mix', 1) in self.phases or ('ffn', 1) in self.phases) \
            and self.phases.index(('ffn', 0)) < min(self.phases.index(p) for p in self.phases if p[1] == 1)
        if not self.mod_deferred:
            self.mod_layer(1, wb, 512)
        P.barrier()

    def mod_layer(self, l, wb, W):
        for _ in self.mod_layer_gen(l, wb, W):
            pass

    def mod_layer_gen(self, l, wb, W):
        P = self.P
        pm = self.PB[0]
        nsub = W // 128
        nblk = 6 * D // W

        def load(blk):
            w = wb[blk % 2]
            P.dma_cast(w.t[:, :, 0:W], self.ada_w[l, :, blk * W:(blk + 1) * W].rearrange("(kc p) n -> p kc n", p=128),
                       writes=[w])

        load(0)
        for blk in range(nblk):
            if blk + 1 < nblk:
                load(blk + 1)
            yield
            w = wb[blk % 2]
            for oc in range(nsub):
                j = blk * nsub + oc
                o = pm.t[:, oc * NSEQ:(oc + 1) * NSEQ]
                for kc in range(8):
                    MM(P, pm, o, w.t[:, kc, oc * 128:(oc + 1) * 128], self.cact.t[:, kc, :], [w, self.cact],
                       kc == 0, kc == 7)
                ACT(P, self.modv, self.modv.t[:, l, j, :], o, AF.Identity, [pm, self.cf],
                    bias=self.c('ada_b', l * 48 + j, 1))
        for b in range(NSEQ):
            g = lambda w_: self.c('gains', (l * 4 + w_) * 8, 8)
            d = self.drv
            STT(P, 'dve', d, d.t[:, l, b, 0, :], self.modv.t[:, l, 8:16, b], 1.0, g(0), ALU.add, ALU.mult,
                [self.modv, self.cf])
            TT(P, 'dve', d, d.t[:, l, b, 1, :], self.modv.t[:, l, 16:24, b], g(1), ALU.mult, [self.modv, self.cf])
            STT(P, 'dve', d, d.t[:, l, b, 2, :], self.modv.t[:, l, 32:40, b], 1.0, g(2), ALU.add, ALU.mult,
                [self.modv, self.cf])
            TT(P, 'dve', d, d.t[:, l, b, 3, :], self.modv.t[:, l, 40:48, b], g(3), ALU.mult, [self.modv, self.cf])

    def rsq(self, buf, ap):
        P = self.P
        ACT(P, buf, ap, ap, AF.Sqrt, [buf])
        P.op('dve', lambda e: e.reciprocal(ap, ap), [buf], [buf])

    def rms_rstd(self, get_src, n, W, sqb, rstd):
        P = self.P
        pb = self.PB[0]
        for c in range(n):
            sb_, sap = get_src(c)
            q = sqb[c % 2]
            ACT(P, q, q.t[:, 0:W], sap, AF.Square, [sb_])
            MM(P, pb, pb.t[:, 0:W], self.ones.t[:], q.t[:, 0:W], [q, self.ones], c == 0, c == n - 1)
        TS(P, 'dve', rstd, rstd.t[:, 0:W], pb.t[:, 0:W], 1.0 / (128 * n), EPS, ALU.mult, ALU.add, [pb])
        self.rsq(rstd, rstd.t[:, 0:W])

    def prenorm(self, s, l, which, t0, hm, hoff, sqb, rstd, tmp):
        P = self.P
        xT = self.xTt[t0 // 512]
        self.rms_rstd(lambda c: (xT, xT.t[:, c, t0:t0 + 512]), 8, 512, sqb, rstd)
        Aidx = 0 if which == 0 else 2
        shbase = 0 if which == 0 else 24
        for c in range(8):
            tb = tmp[c % 2]
            STT(P, 'dve', tb, tb.t[:], xT.t[:, c, t0:t0 + 512], self.drv.t[:, l, s, Aidx, c:c + 1], rstd.t[:],
                ALU.mult, ALU.mult, [xT, self.drv, rstd])
            ACT(P, hm, hm.t[:, c, hoff:hoff + 512], tb.t[:], AF.Identity, [tb, self.modv],
                bias=self.modv.t[:, l, shbase + c, s:s + 1])

    def postnorm_residual(self, s, l, which, t0, get_y, W, sqb, rstd, tmp):
        P = self.P
        xT = self.xTt[t0 // 512]
        self.rms_rstd(get_y, 8, W, sqb, rstd)
        Gidx = 1 if which == 0 else 3
        for c in range(8):
            tb = tmp[c % 2]
            yb, yap = get_y(c)
            TT(P, 'dve', tb, tb.t[:, 0:W], yap, rstd.t[:, 0:W], ALU.mult, [yb, rstd])
            STT(P, 'dve', xT, xT.t[:, c, t0:t0 + W], tb.t[:, 0:W], self.drv.t[:, l, s, Gidx, c:c + 1],
                xT.t[:, c, t0:t0 + W], ALU.mult, ALU.add, [tb, self.drv, xT])

    def group_norm_tm(self, l, src, src_ap, chunk0, mixT, tokoff, tmps):
        P = self.P
        junk, ssq, on = tmps
        ACT(P, junk, junk.t[:, 256:512], src_ap, AF.Square, [src], acc=ssq.t[:, 0:1], wr=[junk, ssq])
        TS(P, 'dve', ssq, ssq.t[:, 1:2], ssq.t[:, 0:1], 1.0 / 256, EPS, ALU.mult, ALU.add, [ssq])
        self.rsq(ssq, ssq.t[:, 1:2])
        TS(P, 'dve', on, on.t[:, 0:256], src_ap, ssq.t[:, 1:2], None, ALU.mult, None, [src, ssq])
        pt = self.PB[7]
        ptv = pt.t[:].bitcast(BF16)
        for cc in range(2):
            TR(P, pt, ptv[:, cc * 128:(cc + 1) * 128], on.t[:, cc * 128:(cc + 1) * 128], self.ident.t[:],
               [on, self.ident])
            ACT(P, mixT, mixT.t[:, chunk0 + cc, tokoff:tokoff + 128], ptv[:, cc * 128:(cc + 1) * 128], AF.Identity,
                [pt, self.cf], scale=self.c('ggain', l * 8 + chunk0 + cc, 1))

    def mixer(self, s, l):
        P = self.P
        ar = self.ar
        ar.reset()
        cv = ar.carve
        hm = cv("hm", [8, 512], BF16)
        mixT = cv("mixT", [8, 512], BF16)
        wreg = cv("wreg", [8, 1024], BF16)
        wblk = [Buf("wblk0", wreg.t[:, :, 0:512]), Buf("wblk1", wreg.t[:, :, 512:1024])]
        qT = cv("qT", [4, 512], BF16)
        kT = cv("kT", [4, S], BF16)
        vaug = cv("vaug", [16, 8, 65], BF16)
        glu = cv("glu", [2, 542], F32)
        biasT = cv("biasT", [4, 512], BF16, parts=8)
        posi = cv("posi", [512], I32)
        cosT = cv("cosT", [512], F32)
        sinS = cv("sinS", [512], F32)
        sqb = [cv("sq%d" % i, [512], BF16) for i in range(2)]
        rstd = cv("rstd", [512], F32)
        tmp = [cv("tmp%d" % i, [512], F32) for i in range(4)]
        PT = [cv("PT%d" % i, [512], BF16) for i in range(5)]
        gl = cv("gl", [512], F32)
        vgn = cv("vgn", [256], BF16)
        wsTm = cv("wsTm", [4, 128], BF16)
        bb = cv("bb", [256], F32)
        stt = cv("stt", [8], F32)
        ssq = cv("ssq", [4], F32)
        on = cv("on", [256], BF16)
        ocb = cv("ocb", [256], F32)
        og = cv("og", [4, 256], F32)
        rec = cv("rec", [4], F32)
        kms = cv("kms", [2, 8], F32)
        kmb = cv("kmb", [2, 8], BF16)
        gm = cv("gm", [32], F32)
        top8 = cv("top8", [4, 8], F32)
        sel = cv("sel", [32], F32)
        bq = cv("bq", [32], BF16)
        acc = [cv("acc%d" % i, [512], F32) for i in range(2)]
        ybf = cv("ybf", [2, 512], BF16)
        PB = self.PB
        cf = self.cf
        gnt = (gl, ssq, on)

        P.dma_cast(wsTm.t[:], self.wsT[l].rearrange("g s t -> s g t"), writes=[wsTm])
        for g in range(4):
            TT(P, 'dve', wsTm, wsTm.t[:, g, :], wsTm.t[:, g, :], self.tri.t[:], ALU.mult, [wsTm, self.tri])
            TS(P, 'dve', bb, bb.t[:, g * 64:(g + 1) * 64], self.c('ones'), self.c('gb', l * 4 + g, 1), None, ALU.mult,
               None, [cf])
        P.op('dve', lambda e: e.memset(vaug.t[:, :, :, 64:65], 1.0), (), [vaug])
        P.op('dve', lambda e: e.memset(glu.t[:, :, 0:30], 0.0), (), [glu])
        P.op('dve', lambda e: e.memset(kms.t[:], 0.0), (), [kms])

        wsrc = self.w_in[l]
        state = {'issued': 0}

        def issue_to(n):
            while state['issued'] < n:
                i = state['issued']
                b_ = i % 7
                w = wblk[i % 2]
                P.dma_cast(w.t[:], wsrc[:, b_ * 512:(b_ + 1) * 512].rearrange("(kc p) n -> p kc n", p=128), writes=[w])
                state['issued'] += 1

        def prep(tt):
            t0 = tt * 512
            posf, angt = tmp[2], tmp[3]
            fi_, gg_ = tmp[0], tmp[1]
            posi2 = Buf("posi2", posi.t)
            P.dma(posi.t[:], self.posrep[s, :, t0:t0 + 512], writes=[posi])
            CP(P, 'dve', posf, posf.t[:], posi.t[:], [posi])
            TS(P, 'dve', posf, posf.t[:], posf.t[:], self.c('invf'), None, ALU.mult, None, [posf, cf])
            CP(P, 'dve', posi, posi.t[:], posf.t[:], [posf])
            CP(P, 'dve', fi_, fi_.t[:], posi.t[:], [posi])
            TT(P, 'dve', angt, angt.t[:], posf.t[:], fi_.t[:], ALU.subtract, [posf, fi_])
            TS(P, 'dve', gg_, gg_.t[:], angt.t[:], 0.5, None, ALU.is_gt, None, [angt])
            TT(P, 'dve', angt, angt.t[:], angt.t[:], gg_.t[:], ALU.subtract, [angt, gg_])
            TS(P, 'dve', gg_, gg_.t[:], angt.t[:], -0.5, None, ALU.is_lt, None, [angt])
            TT(P, 'dve', angt, angt.t[:], angt.t[:], gg_.t[:], ALU.add, [angt, gg_])
            ACT(P, sinS, sinS.t[:], angt.t[:], AF.Sin, [angt, cf], scale=self.c('sgn'))
            TS(P, 'dve', angt, angt.t[:], angt.t[:], 0.25, None, ALU.add, None, [angt])
            TS(P, 'dve', gg_, gg_.t[:], angt.t[:], 0.5, None, ALU.is_gt, None, [angt])
            TT(P, 'dve', angt, angt.t[:], angt.t[:], gg_.t[:], ALU.subtract, [angt, gg_])
            ACT(P, cosT, cosT.t[:], angt.t[:], AF.Sin, [angt], scale=6.28318)
            self.prenorm(s, l, 0, t0, hm, 0, sqb, rstd, tmp)

        prep(0)
        for tt in range(4):
            t0 = tt * 512
            if self.dbg_on and tt == 0:
                self.tap("cos", cosT, cosT.t[:], [128, 512])
                self.tap("sin", sinS, sinS.t[:], [128, 512])
            for b in range(7):
                bi = tt * 7 + b
                issue_to(min(bi + 2, (tt + 1) * 7))
                w = wblk[bi % 2]
                if b < 5:
                    for cc in range(2):
                        pa = PB[1 + 2 * self.nxt('pp', 2)]
                        pbk = PB[2] if pa is PB[1] else PB[4]
                        for kc in range(8):
                            MM(P, pa, pa.t[:], w.t[:, kc, cc * 128:(cc + 1) * 128], hm.t[:, kc, :], [w, hm], kc == 0, kc == 7)
                        for kc in range(8):
                            MM(P, pbk, pbk.t[:], w.t[:, kc, 256 + cc * 128:256 + (cc + 1) * 128], hm.t[:, kc, :], [w, hm],
                               kc == 0, kc == 7)
                        if b < 4:
                            t1, t2 = tmp[0], tmp[1]
                            TT(P, 'dve', t1, t1.t[:], pa.t[:], cosT.t[:], ALU.mult, [pa, cosT])
                            TT(P, 'dve', t2, t2.t[:], pbk.t[:], sinS.t[:], ALU.mult, [pbk, sinS])
                            if b % 2 == 0:
                                dst, dap = qT, qT.t[:, (b // 2) * 2 + cc, :]
                            else:
                                dst, dap = kT, kT.t[:, (b // 2) * 2 + cc, t0:t0 + 512]
                            TT(P, 'pool', dst, dap, t1.t[:], t2.t[:], ALU.add, [t1, t2])
                        else:
                            t1 = tmp[2]
                            ACT(P, t1, t1.t[:], pbk.t[:], AF.Sigmoid, [pbk])
                            TT(P, 'dve', glu, glu.t[:, cc, 30:542], pa.t[:], t1.t[:], ALU.mult, [pa, t1])
                else:
                    for sub in range(4):
                        pc = PB[5 + self.nxt('pc', 2)]
                        for kc in range(8):
                            MM(P, pc, pc.t[:], hm.t[:, kc, sub * 128:(sub + 1) * 128], w.t[:, kc, :], [w, hm], kc == 0, kc == 7)
                        if b == 5:
                            ACT(P, vaug, vaug.t[:, tt * 4 + sub, :, 0:64], pc.t[:].rearrange("p (h d) -> p h d", h=8),
                                AF.Copy, [pc])
                        else:
                            self.cpath(l, pc, sub, mixT, gl, vgn, wsTm, bb, stt, gnt, ocb, tmp)
            P.dma_cast(wreg.t[:], self.w_out[l].rearrange("(kc p) n -> p kc n", p=128), writes=[wblk[0], wblk[1]])
            if tt + 1 < 4:
                prep(tt + 1)
            if self.dbg_on and tt == 0:
                self.tap("q", qT, qT.t[:], [128, 4, 512], BF16)
                self.tap("k", kT, kT.t[:, :, 0:512], [128, 4, 512], BF16)
                self.tap("glu", glu, glu.t[:], [128, 2, 542])
                self.tap("v", vaug, vaug.t[:, 0:4, :, :], [128, 4, 8, 65], BF16)
            self.gating(tt, qT, kT, kms, kmb, gm, top8, sel, bq, biasT)
            dp = self.dpath(l, glu, acc, ybf, rstd, tmp, mixT)
            if self.dbg_on and tt == 1:
                self.tap("biasT", biasT, biasT.t[:], [8, 4, 512], BF16)
            for G in range(2):
                self.attn_group(G, tt, qT, kT, vaug, biasT, PT, og, rec, dp)
                if self.dbg_on and tt == 1:
                    self.tap("og%d" % G, og, og.t[:], [128, 4, 256])
                for qs in range(4):
                    self.group_norm_tm(l, og, og.t[:, qs, :], G * 2, mixT, qs * 128, gnt)
            for _ in dp:
                pass
            if self.dbg_on and tt == 1:
                self.tap("mixT", mixT, mixT.t[:], [128, 8, 512], BF16)
            for hf in range(2):
                c0 = hf * 256

                def ysrc(oc):
                    return PB[1 + oc // 2], PB[1 + oc // 2].t[:, (oc % 2) * 256:(oc % 2) * 256 + 256]
                for oc in range(8):
                    yb, yap = ysrc(oc)
                    for kc in range(8):
                        MM(P, yb, yap, wreg.t[:, kc, oc * 128:(oc + 1) * 128], mixT.t[:, kc, c0:c0 + 256],
                           [wblk[0], wblk[1], mixT], kc == 0, kc == 7)
                self.postnorm_residual(s, l, 0, t0 + c0, ysrc, 256, sqb, rstd, tmp)
        P.barrier()

    def cpath(self, l, pc, sub, mixT, gl, vgn, wsTm, bb, stt, gn_tmps, ocb, tmp):
        P = self.P
        tA, tB = tmp[2], tmp[3]
        ACT(P, tA, tA.t[:], pc.t[:], AF.Square, [pc])
        TS(P, 'pool', tA, tA.t[:], tA.t[:], 0.044715, 1.0, ALU.mult, ALU.add, [tA])
        TT(P, 'dve', tA, tA.t[:], tA.t[:], pc.t[:], ALU.mult, [tA, pc])
        ACT(P, tB, tB.t[:], tA.t[:], AF.Sigmoid, [tA], scale=1.5957691216057308)
        TT(P, 'dve', gl, gl.t[:], pc.t[:], tB.t[:], ALU.mult, [pc, tB])
        P.op('dve', lambda e: e.bn_stats(stt.t[:, 0:6], gl.t[:, 256:512]), [gl], [stt])
        P.op('dve', lambda e: e.bn_aggr(stt.t[:, 6:8], stt.t[:, 0:6]), [stt], [stt])
        TS(P, 'dve', stt, stt.t[:, 7:8], stt.t[:, 7:8], EPS, None, ALU.add, None, [stt])
        self.rsq(stt, stt.t[:, 7:8])
        TS(P, 'dve', vgn, vgn.t[:], gl.t[:, 256:512], stt.t[:, 6:7], stt.t[:, 7:8], ALU.subtract, ALU.mult, [gl, stt])
        pz = self.PB[7]
        for g in range(4):
            MM(P, pz, pz.t[:, 256 + g * 64:256 + (g + 1) * 64], wsTm.t[:, g, :], vgn.t[:, g * 64:(g + 1) * 64], [wsTm, vgn])
        TT(P, 'dve', ocb, ocb.t[:], pz.t[:, 256:512], bb.t[:], ALU.add, [pz, bb])
        TT(P, 'dve', ocb, ocb.t[:], ocb.t[:], gl.t[:, 0:256], ALU.mult, [ocb, gl])
        self.group_norm_tm(l, ocb, ocb.t[:], 4, mixT, sub * 128, gn_tmps)

    def dpath(self, l, glu, acc, ybf, rstd, tmp, mixT):
        P = self.P
        cf = self.cf
        PB = self.PB
        for cc in range(2):
            eng = 'dve'
            a = acc[cc]
            wbase = (l * 2 + cc) * 31
            TS(P, eng, a, a.t[:], glu.t[:, cc, 0:512], self.c('convw', wbase, 1), self.c('convv', (l * 3 + 0) * 2 + cc, 1),
               ALU.mult, ALU.add, [glu, cf])
            for j in range(1, 31):
                STT(P, eng, a, a.t[:], glu.t[:, cc, j:j + 512], self.c('convw', wbase + j, 1), a.t[:], ALU.mult, ALU.add,
                    [glu, cf, a])
                if j % 8 == 0:
                    yield
        for cc in range(2):
            CP(P, 'dve', glu, glu.t[:, cc, 0:30], glu.t[:, cc, 512:542], [glu])
        p1, p2 = PB[5], PB[6]
        for cc in range(2):
            ACT(P, ybf, ybf.t[:, cc, :], acc[cc].t[:], AF.Copy, [acc[cc]])
            MM(P, p1, p1.t[:], self.ones.t[:], ybf.t[:, cc, :], [ybf, self.ones], cc == 0, cc == 1)
        for cc in range(2):
            ACT(P, ybf, ybf.t[:, cc, :], acc[cc].t[:], AF.Square, [acc[cc]])
            MM(P, p2, p2.t[:], self.ones.t[:], ybf.t[:, cc, :], [ybf, self.ones], cc == 0, cc == 1)
        t1, mean = tmp[0], tmp[1]
        TS(P, 'dve', mean, mean.t[:], p1.t[:], 1.0 / 256, None, ALU.mult, None, [p1])
        TT(P, 'dve', t1, t1.t[:], mean.t[:], mean.t[:], ALU.mult, [mean])
        STT(P, 'dve', t1, t1.t[:], p2.t[:], 1.0 / 256, t1.t[:], ALU.mult, ALU.subtract, [p2, t1])
        TS(P, 'dve', rstd, rstd.t[:], t1.t[:], EPS, None, ALU.add, None, [t1])
        self.rsq(rstd, rstd.t[:])
        for cc in range(2):
            a = acc[cc]
            TT(P, 'dve', a, a.t[:], a.t[:], mean.t[:], ALU.subtract, [a, mean])
            TT(P, 'dve', a, a.t[:], a.t[:], rstd.t[:], ALU.mult, [a, rstd])
            ACT(P, a, a.t[:], a.t[:], AF.Silu, [a, cf], bias=self.c('convv', (l * 3 + 2) * 2 + cc, 1),
                scale=self.c('convv', (l * 3 + 1) * 2 + cc, 1))
        for cc in range(2):
            ACT(P, ybf, ybf.t[:, cc, :], acc[cc].t[:], AF.Square, [acc[cc]])
            MM(P, p1, p1.t[:], self.ones.t[:], ybf.t[:, cc, :], [ybf, self.ones], cc == 0, cc == 1)
        TS(P, 'dve', rstd, rstd.t[:], p1.t[:], 1.0 / 256, EPS, ALU.mult, ALU.add, [p1])
        self.rsq(rstd, rstd.t[:])
        for cc in range(2):
            a = acc[cc]
            TT(P, 'dve', a, a.t[:], a.t[:], rstd.t[:], ALU.mult, [a, rstd])
            ACT(P, mixT, mixT.t[:, 6 + cc, :], a.t[:], AF.Identity, [a, cf], scale=self.c('ggain', l * 8 + 6 + cc, 1))

    def gating(self, tt, qT, kT, kms, kmb, gm, top8, sel, bq, biasT):
        P = self.P
        cf = self.cf
        for cc in range(2):
            for j in (2 * tt, 2 * tt + 1):
                P.op('dve', lambda e, cc=cc, j=j: e.tensor_reduce(kms.t[:, cc, j:j + 1], kT.t[:, cc, j * 256:(j + 1) * 256],
                                                                  AX.X, ALU.add), [kT], [kms])
        TS(P, 'dve', kmb, kmb.t[:], kms.t[:], 1.0 / 256, None, ALU.mult, None, [kms])
        pg = self.PB[7]
        ptv = pg.t[:].bitcast(BF16)
        for sub in range(4):
            nq = (tt * 4 + sub) // 2
            for h in range(4):
                pb0 = 64 * (h % 2)
                MM(P, pg, pg.t[:, h * 8:(h + 1) * 8], qT.t[pb0:pb0 + 64, h // 2, sub * 128:(sub + 1) * 128],
                   kmb.t[pb0:pb0 + 64, h // 2, :], [qT, kmb])
            TT(P, 'dve', gm, gm.t[:], pg.t[:, 0:32], self.c('pastneg', nq * 32, 32), ALU.add, [pg, cf])
            for h in range(4):
                P.op('dve', lambda e, h=h: e.max(top8.t[:, h, :], gm.t[:, h * 8:(h + 1) * 8]), [gm], [top8])
            for h in range(4):
                TS(P, 'dve', sel, sel.t[:, h * 8:(h + 1) * 8], gm.t[:, h * 8:(h + 1) * 8], top8.t[:, h, 2:3], None, ALU.is_ge,
                   None, [gm, top8])
            TT(P, 'dve', sel, sel.t[:], sel.t[:], self.c('past', nq * 32, 32), ALU.mult, [sel, cf])
            TT(P, 'dve', sel, sel.t[:], sel.t[:], self.c('own', nq * 32, 32), ALU.add, [sel, cf])
            TS(P, 'dve', bq, bq.t[:], sel.t[:], BIG, -BIG, ALU.mult, ALU.add, [sel])
            for h in range(4):
                TR(P, pg, ptv[0:8, 128 + h * 128:128 + (h + 1) * 128], bq.t[:, h * 8:(h + 1) * 8], self.ident.t[:],
                   [bq, self.ident])
            CP(P, 'dve', biasT, biasT.t[:, :, sub * 128:(sub + 1) * 128],
               ptv[0:8, 128:640].rearrange("p (h q) -> p h q", h=4), [pg])

    def attn_group(self, G, qt, qT, kT, vaug, biasT, PT, ogb, rec, dp):
        P = self.P
        PB = self.PB
        nk = 4 * qt + 4
        steps = [(h, kt) for h in range(4) for kt in range(nk)]
        n = len(steps)
        LA = 3
        Sbanks = [PB[1], PB[2], PB[0], PB[7]]
        Es = {}

        def emit_S(i):
            h, kt = steps[i]
            cq = G * 2 + h // 2
            pb0 = 64 * (h % 2)
            Sb = Sbanks[i % 4]
            MM(P, Sb, Sb.t[:], kT.t[pb0:pb0 + 64, cq, kt * 128:(kt + 1) * 128], qT.t[pb0:pb0 + 64, cq, :], [kT, qT],
               True, G == 1)
            if G == 0:
                jb = kt // 2
                MM(P, Sb, Sb.t[:], self.indc.t[0:8, jb * 128:(jb + 1) * 128], biasT.t[0:8, h, :], [self.indc, biasT],
                   False, True)
            E = PT[i % 5]
            ACT(P, E, E.t[:], Sb.t[:], AF.Exp, [Sb], scale=0.125)
            dl = 512 * qt - 128 * kt
            if G == 1:
                mi = (dl + 384) // 128 if dl <= 512 else 8
                TT(P, 'dve', E, E.t[:], E.t[:], self.mB.t[:, mi, :], ALU.mult, [E, self.mB])
            elif dl <= 0:
                TT(P, 'dve', E, E.t[:], E.t[:], self.mA.t[:, (-dl) // 128, :], ALU.mult, [E, self.mA])
            Es[i] = E

        def emit_PV(i):
            h, kt = steps[i]
            O = PB[3 + h % 2]
            E = Es.pop(i)
            for qs in range(4):
                last = 4 * qt + qs
                if kt <= last:
                    MM(P, O, O.t[:, qs * 65:(qs + 1) * 65], E.t[:, qs * 128:(qs + 1) * 128], vaug.t[:, kt, G * 4 + h, :],
                       [E, vaug], kt == 0 and qs == 0, kt == last)
            if kt == nk - 1:
                O3 = O.t[:, 0:260].rearrange("p (q d) -> p q d", q=4)
                P.op('dve', lambda e: e.reciprocal(rec.t[:].rearrange("p (q o) -> p q o", o=1), O3[:, :, 64:65]), [O], [rec])
                for qs in range(4):
                    TS(P, 'dve', ogb, ogb.t[:, qs, h * 64:(h + 1) * 64], O.t[:, qs * 65:qs * 65 + 64], rec.t[:, qs:qs + 1],
                       None, ALU.mult, None, [O, rec])
                next(dp, None)

        for i in range(min(LA, n)):
            emit_S(i)
        for i in range(n):
            if i + LA < n:
                emit_S(i + LA)
            emit_PV(i)

    def ffn(self, s, l, last=False):
        P = self.P
        ar = self.ar
        PB = self.PB
        moe = (l == 1)
        nfb = (DFE if moe else DFF) // 256
        for grp in range(2):
            g0 = grp * 1024
            ar.reset()
            cv = ar.carve
            hT = cv("hT", [8, 1024], BF16)
            yacc = cv("yacc", [8, 1024], F32)
            act = [cv("act%d" % i, [2, 1024], BF16) for i in range(2)]
            wg = [cv("wg%d" % i, [8, 256], BF16) for i in range(2)]
            wu = [cv("wu%d" % i, [8, 256], BF16) for i in range(2)]
            wd = [cv("wd%d" % i, [2, 1024], BF16) for i in range(2)]
            sqb = [cv("sq%d" % i, [512], BF16) for i in range(2)]
            rstd = cv("rstd", [512], F32)
            tmp = [cv("tmp%d" % i, [512], F32) for i in range(4)]
            for tl in range(2):
                self.prenorm(s, l, 1, g0 + tl * 512, hT, tl * 512, sqb, rstd, tmp)
            if (not moe) and s == 0 and grp == 0 and self.mod_deferred:
                wbd = [cv("wadad%d" % i, [8, 512], BF16) for i in range(2)]
                modgen = self.mod_layer_gen(1, wbd, 512)
            else:
                modgen = iter(())
            if moe:
                gatesT = cv("gatesT", [1024], F32, parts=8)
                indf = cv("indf", [1024], F32, parts=8)
                gbc = cv("gbc", [2, 512], F32)
                lg = cv("lg", [8], F32)
                top8 = cv("top8", [8], F32)
                sm = cv("sm", [8], F32)
                e1 = cv("e1", [8], F32)
                e2 = cv("e2", [8], F32)
                P.dma(indf.t[:], self.indc_d, writes=[indf])
                self.router(hT, gatesT, lg, top8, sm, e1, e2)
                blocks = [(e, fb) for e in range(NE) for fb in range(nfb)]
            else:
                blocks = [(0, fb) for fb in range(nfb)]
            nb = len(blocks)

            def wsrcs(i):
                e_, fb_ = blocks[i]
                if moe:
                    return self.moe_wg[e_], self.moe_wu[e_], self.moe_wd[e_], fb_ * 256
                return self.ffn_wg, self.ffn_wu, self.ffn_wd, fb_ * 256

            def issue_gu(i):
                if i >= nb:
                    return
                sg_, su_, sd_, c0 = wsrcs(i)
                P.dma_cast(wg[i % 2].t[:], sg_[:, c0:c0 + 256].rearrange("(kc p) n -> p kc n", p=128), writes=[wg[i % 2]])
                P.dma_cast(wu[i % 2].t[:], su_[:, c0:c0 + 256].rearrange("(kc p) n -> p kc n", p=128), writes=[wu[i % 2]])

            def issue_d(i):
                if i >= nb:
                    return
                sg_, su_, sd_, c0 = wsrcs(i)
                P.dma_cast(wd[i % 2].t[:], sd_[c0:c0 + 256, :].rearrange("(fc p) n -> p fc n", p=128), writes=[wd[i % 2]])

            def gu_units(bi):
                e, fb = blocks[bi]
                wgb, wub, ab = wg[bi % 2], wu[bi % 2], act[bi % 2]
                if moe and fb == 0:
                    for tl in range(2):
                        pgb = PB[0]
                        MM(P, pgb, pgb.t[:], indf.t[0:8, e * 128:(e + 1) * 128], gatesT.t[0:8, tl * 512:(tl + 1) * 512],
                           [indf, gatesT])
                        ACT(P, gbc, gbc.t[:, tl, :], pgb.t[:], AF.Copy, [pgb])
                for fc in range(2):
                    for tl in range(2):
                        pg = PB[1 + 2 * self.nxt('fg', 2)]
                        pu = PB[2] if pg is PB[1] else PB[4]
                        for kc in range(8):
                            MM(P, pg, pg.t[:], wgb.t[:, kc, fc * 128:(fc + 1) * 128], hT.t[:, kc, tl * 512:(tl + 1) * 512],
                               [wgb, hT], kc == 0, kc == 7)
                        for kc in range(8):
                            MM(P, pu, pu.t[:], wub.t[:, kc, fc * 128:(fc + 1) * 128], hT.t[:, kc, tl * 512:(tl + 1) * 512],
                               [wub, hT], kc == 0, kc == 7)
                        t1 = tmp[self.nxt('ft', 2)]
                        ACT(P, t1, t1.t[:], pg.t[:], AF.Silu, [pg])
                        if moe:
                            TT(P, 'dve', t1, t1.t[:], t1.t[:], gbc.t[:, tl, :], ALU.mult, [t1, gbc])
                        TT(P, 'dve', ab, ab.t[:, fc, tl * 512:(tl + 1) * 512], t1.t[:], pu.t[:], ALU.mult, [t1, pu])
                        yield

            def down_parts(bi):
                wdb, ab = wd[bi % 2], act[bi % 2]
                for oc in range(8):
                    for tl in range(2):
                        py = PB[5 + self.nxt('fy', 3)]
                        for fc in range(2):
                            MM(P, py, py.t[:], wdb.t[:, fc, oc * 128:(oc + 1) * 128], ab.t[:, fc, tl * 512:(tl + 1) * 512],
                               [wdb, ab], fc == 0, fc == 1)
                        ya = yacc.t[:, oc, tl * 512:(tl + 1) * 512]
                        if bi == 0:
                            ACT(P, yacc, ya, py.t[:], AF.Copy, [py])
                        else:
                            TT(P, 'dve', yacc, ya, ya, py.t[:], ALU.add, [yacc, py])
                    if oc % 2 == 1:
                        yield

            issue_gu(0)
            issue_d(0)
            issue_gu(1)
            issue_d(1)
            for _ in gu_units(0):
                pass
            for bi in range(nb):
                g_it = gu_units(bi + 1) if bi + 1 < nb else iter(())
                d_it = down_parts(bi)
                issue_gu(bi + 2)
                for u in range(4):
                    next(g_it, None)
                    next(d_it, None)
                for _ in g_it:
                    pass
                for _ in d_it:
                    pass
                issue_d(bi + 2)
                next(modgen, None)
            for _ in modgen:
                pass
            for tl in range(2):
                self.postnorm_residual(s, l, 1, g0 + tl * 512, lambda c, tl=tl: (yacc, yacc.t[:, c, tl * 512:(tl + 1) * 512]),
                                       512, sqb, rstd, tmp)
            P.barrier()
            if last:
                for tile in (2 * grp, 2 * grp + 1):
                    self.xstore(s, tile)
                    if s + 1 < NSEQ:
                        self.xload(s + 1, tile)

    def router(self, hT, gatesT, lg, top8, sm, e1, e2):
        P = self.P
        pl = self.PB[7]
        for sub in range(8):
            for kc in range(8):
                MM(P, pl, pl.t[:, 0:8], hT.t[:, kc, sub * 128:(sub + 1) * 128], self.rwb.t[:, kc, :], [hT, self.rwb],
                   kc == 0, kc == 7)
            TT(P, 'dve', lg, lg.t[:], pl.t[:, 0:8], self.rb.t[:], ALU.add, [pl, self.rb])
            P.op('dve', lambda e: e.max(top8.t[:], lg.t[:]), [lg], [top8])
            TS(P, 'dve', sm, sm.t[:, 0:1], top8.t[:, 1:2], top8.t[:, 0:1], None, ALU.subtract, None, [top8])
            ACT(P, sm, sm.t[:, 1:2], sm.t[:, 0:1], AF.Exp, [sm])
            TS(P, 'dve', sm, sm.t[:, 2:3], sm.t[:, 1:2], 1.0, None, ALU.add, None, [sm])
            P.op('dve', lambda e: e.reciprocal(sm.t[:, 3:4], sm.t[:, 2:3]), [sm], [sm])
            TT(P, 'dve', sm, sm.t[:, 4:5], sm.t[:, 1:2], sm.t[:, 3:4], ALU.mult, [sm])
            TS(P, 'dve', e1, e1.t[:], lg.t[:], top8.t[:, 0:1], sm.t[:, 3:4], ALU.is_equal, ALU.mult, [lg, top8, sm])
            TS(P, 'dve', e2, e2.t[:], lg.t[:], top8.t[:, 1:2], sm.t[:, 4:5], ALU.is_equal, ALU.mult, [lg, top8, sm])
            TT(P, 'dve', e1, e1.t[:], e1.t[:], e2.t[:], ALU.add, [e1, e2])
            TR(P, pl, pl.t[0:8, 128:256], e1.t[:], self.identf.t[:], [e1, self.identf])
            CP(P, 'dve', gatesT, gatesT.t[:, sub * 128:(sub + 1) * 128], pl.t[0:8, 128:256], [pl])

    def xload(self, s, tile):
        a, b = tile * 512, (tile + 1) * 512
        self.P.dma(self.xT.t[:, :, a:b], self.xin[s].rearrange("(c p) t -> p c t", p=128)[:, :, a:b], writes=[self.xTt[tile]])

    def xstore(self, s, tile):
        a, b = tile * 512, (tile + 1) * 512
        self.P.dma(self.xout[s].rearrange("(c p) t -> p c t", p=128)[:, :, a:b], self.xT.t[:, :, a:b], reads=[self.xTt[tile]])

    def build(self):
        P = self.P
        self.setup()
        self.early_io = (self.phases[-1][0] == 'ffn')
        for tile in range(4):
            self.xload(0, tile)
        for s in range(NSEQ):
            if s > 0 and not self.early_io:
                for tile in range(4):
                    self.xload(s, tile)
            for pi, ph in enumerate(self.phases):
                if ph[0] == 'mix':
                    self.mixer(s, ph[1])
                else:
                    self.ffn(s, ph[1], last=(self.early_io and pi == len(self.phases) - 1))
            if not self.early_io:
                for tile in range(4):
                    self.xstore(s, tile)
                P.barrier()
        P.finish()
        return self.nc


def _consts():
    p = np.arange(128)[:, None]
    j = np.arange(512)[None, :]

    def wB(d):
        d = d.astype(np.int64)
        return (((d >= 0) & (d <= 128)).astype(np.float32) + ((d >= 0) & (d % 4 == 0) & (d <= 512)).astype(np.float32)
                + ((d >= 0) & (d % 16 == 0) & (d <= 2048)).astype(np.float32))

    mB = np.zeros((128, 9, 512), np.float32)
    for i in range(8):
        dl = -384 + 128 * i
        mB[:, i, :] = wB(dl + j - p)
    mB[:, 8, :] = ((j - p) % 16 == 0).astype(np.float32)
    mA = np.zeros((128, 4, 512), np.float32)
    for i in range(4):
        mA[:, i, :] = ((-128 * i + j - p) >= 0).astype(np.float32)
    tri = (np.arange(128)[:, None] <= np.arange(128)[None, :]).astype(np.float32)
    indc = np.zeros((8, 8, 128), np.float32)
    for jb in range(8):
        indc[jb, jb, :] = 1.0
    return mB.reshape(128, -1), mA.reshape(128, -1), tri, indc.reshape(8, 1024)


def _prep_shared(inp):
    f = lambda a: np.ascontiguousarray(np.asarray(a, dtype=np.float32))
    sh = {}
    mB, mA, tri, indc = _consts()
    sh.update(mB=mB, mA=mA, tri=tri, indc=indc)
    sh['ada_w'] = f(inp['ada_w'])
    w_in = np.asarray(inp['w_in'], np.float32)
    sl = lambda i: np.arange(i * 256, (i + 1) * 256)
    swp = np.concatenate([np.concatenate([np.arange(h * 64 + 32, h * 64 + 64), np.arange(h * 64, h * 64 + 32)]) for h in range(4)])
    qa, ka, va, qb, kb, vb, u, vg, ga, gg = [sl(i) for i in range(10)]
    cols = np.concatenate([qa, qa[swp], ka, ka[swp], qb, qb[swp], kb, kb[swp], ga, gg, va, vb, u, vg])
    sh['w_in'] = np.ascontiguousarray(w_in[:, :, cols])
    sh['w_out'] = f(inp['w_out'])
    sh['wsT'] = np.ascontiguousarray(np.asarray(inp['gmlp_ws'], np.float32).transpose(0, 1, 3, 2))
    sh['ffn_wg'] = f(inp['ffn_w_gate'][0])
    sh['ffn_wu'] = f(inp['ffn_w_up'][0])
    sh['ffn_wd'] = f(inp['ffn_w_down'][0])
    sh['moe_wg'] = f(inp['moe_w_gate'][0])
    sh['moe_wu'] = f(inp['moe_w_up'][0])
    sh['moe_wd'] = f(inp['moe_w_down'][0])
    sh['rbrep'] = np.ascontiguousarray(np.broadcast_to(np.asarray(inp['router_b'], np.float32)[0][None, :], (128, 8)))
    cf = np.zeros((128, NCF), np.float32)

    def put(name, arr):
        off, n = CM[name]
        cf[:, off:off + n] = np.asarray(arr, np.float32).reshape(128, n)

    half = 32
    invf = np.power(np.float32(10000.0), -np.arange(half, dtype=np.float32) / np.float32(half)).astype(np.float32)
    pidx = np.arange(128)
    put('invf', (invf[pidx % 32].astype(np.float64) / (2 * math.pi)).astype(np.float32))
    sgn = np.where((pidx % 64) < 32, -1.0, 1.0).astype(np.float32)
    put('sgn', (6.28318 * sgn).astype(np.float32))
    put('npsgn', (-math.pi * sgn).astype(np.float32))
    put('negpi', np.full(128, -math.pi, np.float32))
    gains = np.stack([np.asarray(inp[k], np.float32) for k in ('mix_pre_g', 'mix_post_g', 'ffn_pre_g', 'ffn_post_g')], 1)
    put('gains', gains.reshape(2, 4, 8, 128).transpose(3, 0, 1, 2))
    put('ggain', np.asarray(inp['group_out_g'], np.float32).reshape(2, 8, 128).transpose(2, 0, 1))
    put('gb', np.asarray(inp['gmlp_b'], np.float32).transpose(2, 0, 1))
    put('convw', np.asarray(inp['conv_w'], np.float32).reshape(2, 31, 2, 128).transpose(3, 0, 2, 1))
    cvv = np.stack([np.asarray(inp[k], np.float32) for k in ('conv_b', 'conv_ln_g', 'conv_ln_b')], 1)
    put('convv', cvv.reshape(2, 3, 2, 128).transpose(3, 0, 1, 2))
    put('ada_b', np.asarray(inp['ada_b'], np.float32).reshape(2, 48, 128).transpose(2, 0, 1))
    put('rw', np.asarray(inp['router_w'], np.float32)[0].reshape(8, 128, 8).transpose(1, 0, 2))
    n = np.arange(8)[:, None, None]
    jj = np.arange(8)[None, None, :]
    past = np.broadcast_to((jj < n), (8, 4, 8)).astype(np.float32)
    own = np.broadcast_to((jj == n), (8, 4, 8)).astype(np.float32)
    put('past', np.broadcast_to(past.reshape(1, -1), (128, 256)))
    put('own', np.broadcast_to(own.reshape(1, -1), (128, 256)))
    put('pastneg', np.broadcast_to(((1.0 - past) * -1e30).reshape(1, -1), (128, 256)))
    put('ones', np.ones((128, 64), np.float32))
    sh['_cf'] = cf
    return sh


def _core_inputs(inp, sh, core):
    b0 = core * NSEQ
    m = {k: v for k, v in sh.items() if not k.startswith('_')}
    x = np.asarray(inp['x'], np.float32)[b0:b0 + NSEQ]
    m['xT'] = np.ascontiguousarray(x.transpose(0, 2, 1))
    pos = np.asarray(inp['positions'], np.int32)[b0:b0 + NSEQ]
    m['posrep'] = np.ascontiguousarray(np.broadcast_to(pos[:, None, :], (NSEQ, 128, S)))
    cf = sh['_cf'].copy()
    off, n = CM['cT']
    c = np.asarray(inp['c'], np.float32)[b0:b0 + NSEQ]
    cf[:, off:off + n] = c.reshape(NSEQ, 8, 128).transpose(2, 1, 0).reshape(128, n)
    m['cf'] = cf
    return m


ALL_PHASES = [('mix', 0), ('ffn', 0), ('mix', 1), ('ffn', 1)]


def kernel(**inputs):
    sh = _prep_shared(inputs)
    nc = K(ALL_PHASES).build()
    in_maps = [_core_inputs(inputs, sh, c) for c in range(NCORES)]
    res = run_bass_kernel_spmd(nc, in_maps, core_ids=list(range(NCORES)))
    out = np.concatenate([np.asarray(r["outT"], np.float32).transpose(0, 2, 1) for r in res.results], axis=0)
    return np.ascontiguousarray(out.astype(np.float32))
```
